# Optimizing a Trainium2 kernel written in Bass

```python
import numpy as np
import jax
import jax.numpy as jnp
from jax import lax

D_MODEL = 4096
BATCH = 2
SEQ = 8192
DEPTH = 2

GRID_W = 64
CTX_LEN = 256
N_MIXERS = 2
N_A_LAYERS = (DEPTH + 1) // 2
N_B_LAYERS = DEPTH // 2
BLOCK = 128

A_HEAD_DIM = 64
A_HEADS = D_MODEL // A_HEAD_DIM
A_KV_HEADS = 8
A_GROUP = A_HEADS // A_KV_HEADS
WINDOW = 128

B_HEADS = 32
B_Q_RANK = 1024
B_KV_RANK = 512
B_NOPE = 128
B_ROPE = 64
B_V = 128

ROPE_BASE = 10000.0

N_GROUPS = 8
EXPERTS_PER_GROUP = 4
N_EXPERTS = N_GROUPS * EXPERTS_PER_GROUP
TOP_K = 2
D_EXPERT = 512
MOE_BLOCK = 128

EPS = 1e-6
NEG_INF = -1e30

kernel_name = "hybrid_dit_swa_sink_mla_hmoe"


def rmsnorm(x, g):
    xf = x.astype(jnp.float32)
    y = xf * lax.rsqrt(jnp.mean(xf * xf, axis=-1, keepdims=True) + EPS)
    return (y * g.astype(jnp.float32)).astype(x.dtype)


def modulate(x, g, shift, scale):
    xf = x.astype(jnp.float32)
    y = xf * lax.rsqrt(jnp.mean(xf * xf, axis=-1, keepdims=True) + EPS) * g.astype(jnp.float32)
    return (y * (1.0 + scale.astype(jnp.float32)) + shift.astype(jnp.float32)).astype(x.dtype)


def axial_rope(n_tok, d_rot):
    rows = n_tok // GRID_W
    row = jnp.broadcast_to(jnp.arange(rows, dtype=jnp.float32)[:, None], (rows, GRID_W)).reshape(-1)
    col = jnp.broadcast_to(jnp.arange(GRID_W, dtype=jnp.float32)[None, :], (rows, GRID_W)).reshape(-1)
    n_freq = d_rot // 4
    inv = ROPE_BASE ** (-jnp.arange(n_freq, dtype=jnp.float32) / n_freq)
    ang = jnp.concatenate([row[:, None] * inv, col[:, None] * inv], axis=-1)
    return jnp.cos(ang), jnp.sin(ang)


def apply_rope(x, cos, sin):
    half = x.shape[-1] // 2
    xf = x.astype(jnp.float32)
    x1, x2 = xf[..., :half], xf[..., half:]
    return jnp.concatenate([x1 * cos - x2 * sin, x2 * cos + x1 * sin], axis=-1).astype(x.dtype)


def sink_softmax(parts, sink):
    lead = parts[0].shape[:-1]
    sink_col = jnp.broadcast_to(sink.astype(jnp.float32)[None, :, :, None, None], lead + (1,))
    logits = jnp.concatenate([p.astype(jnp.float32) for p in parts] + [sink_col], axis=-1)
    probs = jax.nn.softmax(logits, axis=-1)
    cuts = [int(v) for v in np.cumsum([p.shape[-1] for p in parts])]
    return jnp.split(probs, cuts, axis=-1)[:-1]


def windowed_gqa(h_lat, h_ctx, w_qkv, w_o, sink, with_ctx_out):
    B, S, _ = h_lat.shape
    C = h_ctx.shape[1]
    qd, kvd = A_HEADS * A_HEAD_DIM, A_KV_HEADS * A_HEAD_DIM
    scale = A_HEAD_DIM ** -0.5
    sink_g = sink.reshape(A_KV_HEADS, A_GROUP)
    cos, sin = axial_rope(S, A_HEAD_DIM)
    z = h_lat @ w_qkv
    q = apply_rope(z[..., :qd].reshape(B, S, A_KV_HEADS, A_GROUP, A_HEAD_DIM), cos[:, None, None, :], sin[:, None, None, :])
    k = apply_rope(z[..., qd:qd + kvd].reshape(B, S, A_KV_HEADS, A_HEAD_DIM), cos[:, None, :], sin[:, None, :])
    v = z[..., qd + kvd:].reshape(B, S, A_KV_HEADS, A_HEAD_DIM)
    zc = h_ctx @ (w_qkv if with_ctx_out else w_qkv[:, qd:])
    kc = zc[..., -2 * kvd:-kvd].reshape(B, C, A_KV_HEADS, A_HEAD_DIM)
    vc = zc[..., -kvd:].reshape(B, C, A_KV_HEADS, A_HEAD_DIM)
    nb = S // BLOCK
    pad = ((0, 0), (BLOCK, BLOCK), (0, 0), (0, 0))
    k_pad, v_pad = jnp.pad(k, pad), jnp.pad(v, pad)
    q_blocks = q.reshape(B, nb, BLOCK, A_KV_HEADS, A_GROUP, A_HEAD_DIM).swapaxes(0, 1)
    q_off = jnp.arange(BLOCK)
    k_off = jnp.arange(3 * BLOCK) - BLOCK

    def block(args):
        b, qb = args
        start = b * BLOCK
        kb = lax.dynamic_slice_in_dim(k_pad, start, 3 * BLOCK, axis=1)
        vb = lax.dynamic_slice_in_dim(v_pad, start, 3 * BLOCK, axis=1)
        qpos = start + q_off
        kpos = start + k_off
        valid = (jnp.abs(qpos[:, None] - kpos[None, :]) <= WINDOW) & (kpos >= 0)[None, :] & (kpos < S)[None, :]
        s_loc = jnp.where(valid, jnp.einsum("bqkgd,bjkd->bkgqj", qb, kb).astype(jnp.float32) * scale, NEG_INF)
        s_ctx = jnp.einsum("bqkgd,bckd->bkgqc", qb, kc).astype(jnp.float32) * scale
        p_loc, p_ctx = sink_softmax([s_loc, s_ctx], sink_g)
        return (jnp.einsum("bkgqj,bjkd->bqkgd", p_loc.astype(vb.dtype), vb)
                + jnp.einsum("bkgqc,bckd->bqkgd", p_ctx.astype(vc.dtype), vc))

    o = lax.map(block, (jnp.arange(nb), q_blocks))
    o_lat = o.swapaxes(0, 1).reshape(B, S, qd) @ w_o
    if not with_ctx_out:
        return o_lat, None
    qc = zc[..., :qd].reshape(B, C, A_KV_HEADS, A_GROUP, A_HEAD_DIM)
    (p,) = sink_softmax([jnp.einsum("bqkgd,bckd->bkgqc", qc, kc).astype(jnp.float32) * scale], sink_g)
    o_ctx = jnp.einsum("bkgqc,bckd->bqkgd", p.astype(vc.dtype), vc).reshape(B, C, qd) @ w_o
    return o_lat, o_ctx


def mla(h_lat, h_ctx, w_dkv, g_q, g_kv, w_uq, w_ukv, w_o, with_ctx_out):
    B, S, _ = h_lat.shape
    scale = (B_NOPE + B_ROPE) ** -0.5
    w_ukv3 = w_ukv.reshape(B_KV_RANK, B_HEADS, B_NOPE + B_V)
    w_uk, w_uv = w_ukv3[..., :B_NOPE], w_ukv3[..., B_NOPE:]

    def q_part(zq):
        q = rmsnorm(zq, g_q) @ w_uq
        q = q.reshape(*zq.shape[:-1], B_HEADS, B_NOPE + B_ROPE)
        return q[..., :B_NOPE], q[..., B_NOPE:]

    def kv_part(zkv):
        return rmsnorm(zkv[..., :B_KV_RANK], g_kv), zkv[..., B_KV_RANK:]

    def attend(qn, qr, ckv, kr):
        q_lat = jnp.einsum("bqhn,lhn->bqhl", qn, w_uk)
        s = jnp.einsum("bqhl,bkl->bhqk", q_lat, ckv) + jnp.einsum("bqhr,bkr->bhqk", qr, kr)
        p = jax.nn.softmax(s.astype(jnp.float32) * scale, axis=-1).astype(ckv.dtype)
        o_lat = jnp.einsum("bhqk,bkl->bqhl", p, ckv)
        o = jnp.einsum("bqhl,lhv->bqhv", o_lat, w_uv)
        return o.reshape(*o.shape[:2], B_HEADS * B_V)

    cos, sin = axial_rope(S, B_ROPE)
    z = h_lat @ w_dkv
    qn, qr = q_part(z[..., :B_Q_RANK])
    qr = apply_rope(qr, cos[:, None, :], sin[:, None, :])
    ckv, kr = kv_part(z[..., B_Q_RANK:])
    kr = apply_rope(kr, cos, sin)
    zc = h_ctx @ (w_dkv if with_ctx_out else w_dkv[:, B_Q_RANK:])
    ckv_c, kr_c = kv_part(zc[..., -(B_KV_RANK + B_ROPE):])
    ckv_all = jnp.concatenate([ckv, ckv_c], axis=1)
    kr_all = jnp.concatenate([kr, kr_c], axis=1)
    nb = S // BLOCK
    qn_b = qn.reshape(B, nb, BLOCK, B_HEADS, B_NOPE).swapaxes(0, 1)
    qr_b = qr.reshape(B, nb, BLOCK, B_HEADS, B_ROPE).swapaxes(0, 1)
    o = lax.map(lambda a: attend(a[0], a[1], ckv_all, kr_all), (qn_b, qr_b))
    o_lat = o.swapaxes(0, 1).reshape(B, S, B_HEADS * B_V) @ w_o
    if not with_ctx_out:
        return o_lat, None
    qn_c, qr_c = q_part(zc[..., :B_Q_RANK])
    o_ctx = attend(qn_c, qr_c, ckv_c, kr_c) @ w_o
    return o_lat, o_ctx


def hier_moe(h, w_rg, b_rg, w_re, b_re, w_gu, w_dn):
    T, D = h.shape
    hf = h.astype(jnp.float32)
    lg = hf @ w_rg.astype(jnp.float32) + b_rg.astype(jnp.float32)
    grp = jnp.argmax(lg, axis=-1)
    p_grp = jnp.take_along_axis(jax.nn.softmax(lg, axis=-1), grp[:, None], axis=-1)
    le = (hf @ w_re.astype(jnp.float32) + b_re.astype(jnp.float32)).reshape(T, N_GROUPS, EXPERTS_PER_GROUP)
    le = jnp.take_along_axis(le, grp[:, None, None], axis=1)[:, 0]
    p_top, e_top = lax.top_k(jax.nn.softmax(le, axis=-1), TOP_K)
    gate = p_grp * p_top / jnp.sum(p_top, axis=-1, keepdims=True)
    eid = (grp[:, None] * EXPERTS_PER_GROUP + e_top).reshape(-1)
    n_assign = T * TOP_K
    order = jnp.argsort(eid)
    e_sorted = eid[order]
    tok_sorted = order // TOP_K
    w_sorted = gate.reshape(-1)[order]
    counts = jnp.zeros((N_EXPERTS,), jnp.int32).at[eid].add(1)
    padded = (counts + MOE_BLOCK - 1) // MOE_BLOCK * MOE_BLOCK
    p_end = jnp.cumsum(padded)
    dest = (p_end - padded)[e_sorted] + jnp.arange(n_assign) - (jnp.cumsum(counts) - counts)[e_sorted]
    n_rows = (n_assign + N_EXPERTS * (MOE_BLOCK - 1) + MOE_BLOCK - 1) // MOE_BLOCK * MOE_BLOCK
    n_blocks = n_rows // MOE_BLOCK
    buf = jnp.zeros((n_rows, D), h.dtype).at[dest].set(h[tok_sorted])
    blk_e = jnp.minimum(jnp.searchsorted(p_end, jnp.arange(n_blocks) * MOE_BLOCK, side="right"), N_EXPERTS - 1)

    def expert_block(args):
        xb, e = args
        gu = xb @ w_gu[e]
        return (jax.nn.silu(gu[:, :D_EXPERT]) * gu[:, D_EXPERT:]) @ w_dn[e]

    y_buf = lax.map(expert_block, (buf.reshape(n_blocks, MOE_BLOCK, D), blk_e)).reshape(n_rows, D)
    y = jnp.zeros((T, D), jnp.float32).at[tok_sorted].add(y_buf[dest].astype(jnp.float32) * w_sorted[:, None])
    return y.astype(h.dtype)


def setup_inputs(seed: int = 0) -> dict:
    key = jax.random.key(seed)
    ks = jax.random.split(key, 24)
    D = D_MODEL

    def nrm(k, shape, s=1.0):
        return jax.random.normal(k, shape, jnp.float32) * s

    qkv_cols = (A_HEADS + 2 * A_KV_HEADS) * A_HEAD_DIM
    return {
        "x": nrm(ks[0], (BATCH, SEQ, D)),
        "c": nrm(ks[1], (BATCH, D)),
        "ctx": nrm(ks[2], (BATCH, CTX_LEN, D)),
        "c_ctx": nrm(ks[3], (D,)),
        "ada_w": nrm(ks[4], (DEPTH, D, 6 * D), 0.5 * D ** -0.5),
        "ada_b": nrm(ks[5], (DEPTH, 6 * D), 0.02),
        "norm_g": 1.0 + nrm(ks[6], (DEPTH, 2, D), 0.02),
        "final_g": 1.0 + nrm(ks[7], (D,), 0.02),
        "a_wqkv": nrm(ks[8], (N_A_LAYERS, D, qkv_cols), D ** -0.5),
        "a_wo": nrm(ks[9], (N_A_LAYERS, A_HEADS * A_HEAD_DIM, D), (A_HEADS * A_HEAD_DIM) ** -0.5),
        "a_sink": nrm(ks[10], (N_A_LAYERS, A_HEADS), 1.0),
        "b_wdkv": nrm(ks[11], (N_B_LAYERS, D, B_Q_RANK + B_KV_RANK + B_ROPE), D ** -0.5),
        "b_gq": 1.0 + nrm(ks[12], (N_B_LAYERS, B_Q_RANK), 0.02),
        "b_gkv": 1.0 + nrm(ks[13], (N_B_LAYERS, B_KV_RANK), 0.02),
        "b_wuq": nrm(ks[14], (N_B_LAYERS, B_Q_RANK, B_HEADS * (B_NOPE + B_ROPE)), B_Q_RANK ** -0.5),
        "b_wukv": nrm(ks[15], (N_B_LAYERS, B_KV_RANK, B_HEADS * (B_NOPE + B_V)), B_KV_RANK ** -0.5),
        "b_wo": nrm(ks[16], (N_B_LAYERS, B_HEADS * B_V, D), (B_HEADS * B_V) ** -0.5),
        "r_wg": nrm(ks[17], (DEPTH, D, N_GROUPS), D ** -0.5),
        "r_bg": nrm(ks[18], (DEPTH, N_GROUPS), 0.01),
        "r_we": nrm(ks[19], (DEPTH, D, N_EXPERTS), D ** -0.5),
        "r_be": nrm(ks[20], (DEPTH, N_EXPERTS), 0.01),
        "e_wgu": nrm(ks[21], (DEPTH, N_EXPERTS, D, 2 * D_EXPERT), D ** -0.5),
        "e_wdn": nrm(ks[22], (DEPTH, N_EXPERTS, D_EXPERT, D), D_EXPERT ** -0.5),
    }


def reference(x, c, ctx, c_ctx, ada_w, ada_b, norm_g, final_g, a_wqkv, a_wo, a_sink,
              b_wdkv, b_gq, b_gkv, b_wuq, b_wukv, b_wo, r_wg, r_bg, r_we, r_be, e_wgu, e_wdn):
    B, S, D = x.shape
    C = ctx.shape[1]
    x_lat, x_ctx = x, ctx
    silu_c = jax.nn.silu(c)
    silu_cc = jax.nn.silu(c_ctx)
    for i in range(DEPTH):
        last = i == DEPTH - 1
        j = i // N_MIXERS
        mod_lat = (silu_c @ ada_w[i] + ada_b[i])[:, None, :]
        sh1, sc1, g1, sh2, sc2, g2 = jnp.split(mod_lat, 6, axis=-1)
        n_ctx_mod = 3 if last else 6
        mod_ctx = silu_cc @ ada_w[i][:, :n_ctx_mod * D] + ada_b[i][:n_ctx_mod * D]
        cm = jnp.split(mod_ctx, n_ctx_mod)
        h_lat = modulate(x_lat, norm_g[i, 0], sh1, sc1)
        h_ctx = modulate(x_ctx, norm_g[i, 0], cm[0], cm[1])
        if i % N_MIXERS == 0:
            o_lat, o_ctx = windowed_gqa(h_lat, h_ctx, a_wqkv[j], a_wo[j], a_sink[j], not last)
        else:
            o_lat, o_ctx = mla(h_lat, h_ctx, b_wdkv[j], b_gq[j], b_gkv[j], b_wuq[j], b_wukv[j], b_wo[j], not last)
        x_lat = x_lat + g1 * o_lat
        h_lat2 = modulate(x_lat, norm_g[i, 1], sh2, sc2)
        if last:
            y = hier_moe(h_lat2.reshape(B * S, D), r_wg[i], r_bg[i], r_we[i], r_be[i], e_wgu[i], e_wdn[i])
            x_lat = x_lat + g2 * y.reshape(B, S, D)
        else:
            x_ctx = x_ctx + cm[2] * o_ctx
            h_ctx2 = modulate(x_ctx, norm_g[i, 1], cm[3], cm[4])
            tokens = jnp.concatenate([h_lat2.reshape(B * S, D), h_ctx2.reshape(B * C, D)], axis=0)
            y = hier_moe(tokens, r_wg[i], r_bg[i], r_we[i], r_be[i], e_wgu[i], e_wdn[i])
            x_lat = x_lat + g2 * y[:B * S].reshape(B, S, D)
            x_ctx = x_ctx + cm[5] * y[B * S:].reshape(B, C, D)
    return rmsnorm(x_lat, final_g)
```

```python
import os
import numpy as np
from contextlib import ExitStack
import concourse.bass as bass
import concourse.mybir as mybir
from concourse.bass_utils import run_bass_kernel_spmd

F32 = mybir.dt.float32
BF16 = mybir.dt.bfloat16
AF = mybir.ActivationFunctionType
ALU = mybir.AluOpType
AX = mybir.AxisListType

D = 4096
KC = 32
EPS = 1e-6
NEGM = -30000.0


class Res:
    def __init__(self):
        self.w = None
        self.r = {}


class Tile(Res):
    def __init__(self, t):
        super().__init__()
        self.t = t

    def __getitem__(self, k):
        return self.t[k]


class Tview:
    def __init__(self, tile, ncol):
        self.tile = tile
        self.ncol = ncol

    def __getitem__(self, k):
        return self.tile.t[k[0], k[1], 0:self.ncol]


class Tr:
    def __init__(self, nc, st):
        self.nc = nc
        self.wcache = {}
        self.sems = []

        def mk(name):
            self.sems.append(st.enter_context(nc.semaphore(name)))
            return len(self.sems) - 1

        self.E = {}
        for nm, eng in (("pe", nc.tensor), ("act", nc.scalar), ("dve", nc.vector),
                        ("pool", nc.gpsimd), ("sp", nc.sync)):
            self.E[nm] = dict(eng=eng, sem=mk("s_" + nm), cnt=0, known={})
        self.dq = {}
        for q, n in (("sp", 8), ("pool", 8), ("act", 4)):
            self.dq[q] = dict(sl=[[mk("d_%s%d" % (q, i)), 0] for i in range(n)], i=0)

    def _wait(self, e, toks):
        E = self.E[e]
        for (sm, v) in toks:
            if E["known"].get(sm, 0) < v:
                E["eng"].wait_ge(self.sems[sm], v)
                E["known"][sm] = v

    def _deps(self, e, reads, writes):
        toks = []
        for r in reads:
            if r.w:
                toks.append(r.w)
        for w in writes:
            if w.w:
                toks.append(w.w)
            toks.extend(w.r.items())
        if e == "pe":
            ps = self.E["pe"]["sem"]
            toks = [t for t in toks if t[0] != ps]
        return toks

    def _mark(self, tok, reads, writes):
        for r in reads:
            if r.r.get(tok[0], 0) < tok[1]:
                r.r[tok[0]] = tok[1]
        for w in writes:
            w.w = tok
            w.r = {}

    def op(self, e, fn, reads=(), writes=()):
        writes = list(writes) + [r for r in reads if getattr(r, "excl", False) and r not in writes]
        self._wait(e, self._deps(e, reads, writes))
        E = self.E[e]
        ins = fn(E["eng"])
        E["cnt"] += 1
        ins.then_inc(self.sems[E["sem"]], 1)
        self._mark((E["sem"], E["cnt"]), reads, writes)

    def dma(self, q, out, in_, reads=(), writes=(), **kw):
        Q = self.dq[q]
        sl = Q["sl"][Q["i"] % len(Q["sl"])]
        Q["i"] += 1
        toks = self._deps(q, reads, writes) + [(sl[0], sl[1])]
        self._wait(q, toks)
        self.E[q]["eng"].dma_start(out=out, in_=in_, **kw).then_inc(self.sems[sl[0]], 16)
        sl[1] += 16
        self._mark((sl[0], sl[1]), reads, writes)

    def wload(self, q, tile, src, n1, step=4, key=None):
        if isinstance(tile, Tview) or key is None:
            res = tile.tile if isinstance(tile, Tview) else tile
            self.dma(q, tile[:, 0:n1], src[:, 0:n1], writes=[res])
            return
        flat = tile.t[:].rearrange("p a b -> p (a b)")
        if key in self.wcache:
            cap, cres = self.wcache[key]
            self.dma(q, flat, cap, reads=[cres], writes=[tile])
            return
        self.dma(q, tile[:, 0:n1], src[:, 0:n1], writes=[tile])
        nfree = 1
        for d_ in tile.t.shape[1:]:
            nfree *= d_
        cap = self.nc.dram_tensor("wc_%d" % len(self.wcache), [128, nfree], BF16).ap()
        cres = Res()
        self.wcache[key] = (cap, cres)
        self.dma("sp", cap, flat, reads=[tile], writes=[cres])

    def barrier(self):
        toks = [(E["sem"], E["cnt"]) for E in self.E.values() if E["cnt"] > 0]
        for Q in self.dq.values():
            toks += [(sl[0], sl[1]) for sl in Q["sl"] if sl[1] > 0]
        for e in self.E:
            self._wait(e, toks)


def build(T, C, NE, dbg=False, upto=99, modv_in=False):
    NG = NE // 4
    NTOK = T + C
    nc = bass.Bass("TRN2", target_bir_lowering=False)

    def din(name, shape):
        return nc.dram_tensor(name, list(shape), F32, kind="ExternalInput").ap()

    x_in = din("x", (T, D))
    c_in = din("c", (D,))
    ctx_in = din("ctx", (C, D))
    cctx_in = din("c_ctx", (D,))
    if modv_in:
        modv_src = din("modv_in", (2, 2, 6 * D))
    else:
        ada_w = din("ada_w", (2, D, 6 * D))
        ada_b = din("ada_b", (2, 6 * D))
    norm_g = din("norm_g", (2, 2, D))
    final_g = din("final_g", (D,))
    a_wqkv = din("a_wqkv", (D, 5120))
    a_wo = din("a_wo", (D, D))
    a_sink = din("a_sink", (64,))
    b_wdkv = din("b_wdkv", (D, 1600))
    b_gq = din("b_gq", (1024,))
    b_gkv = din("b_gkv", (512,))
    b_wuq = din("b_wuq", (1024, 6144))
    b_wukv = din("b_wukv", (512, 8192))
    b_wo = din("b_wo", (D, D))
    NR = NG + NE
    r_w = din("r_w", (2, D, NR))
    r_b = din("r_b", (2, NR))
    e_wgu = din("e_wgu", (2, NE, D, 1024))
    e_wdn = din("e_wdn", (2, NE, 512, D))
    k_ident = din("k_ident", (128, 128))
    k_rot = din("k_rot", (128, 128))
    k_cos = din("k_cos", (128, T))
    k_sin = din("k_sin", (128, T))
    k_mprev = din("k_mprev", (128, 512))
    k_mnext = din("k_mnext", (128, 512))
    out = nc.dram_tensor("out", [T, D], F32, kind="ExternalOutput").ap()

    def dscr(name, shape, dt):
        if dbg:
            return nc.dram_tensor(name, list(shape), dt, kind="ExternalOutput").ap()
        return nc.dram_tensor(name, list(shape), dt).ap()

    xres = dscr("xres", (NTOK, D), F32)
    modv = dscr("modv", (2, 2, 6 * D), F32)
    hT = dscr("hT", (D, NTOK), BF16)
    qT = dscr("qT", (D, NTOK), BF16)
    kT = dscr("kT", (512, NTOK), BF16)
    vv = dscr("vv", (NTOK, 512), BF16)
    oT = dscr("oT", (D, NTOK), BF16)
    zqT = dscr("zqT", (1024, T), BF16)
    ckvT = dscr("ckvT", (512, NTOK), BF16)
    krT = dscr("krT", (64, NTOK), BF16)
    qnT = dscr("qnT", (32 * 128, T), BF16)
    qrT = dscr("qrT", (32 * 64, T), BF16)

    st = ExitStack()
    with st:
        tr = Tr(nc, st)
        op, dma = tr.op, tr.dma

        uid = [0]

        def sb(ph, name, shape, dt):
            uid[0] += 1
            return Tile(ph.enter_context(nc.sbuf_tensor("%s_%d" % (name, uid[0]), list(shape), dt)))

        PS = [Tile(st.enter_context(nc.psum_tensor("ps%d" % i, [128, 512], F32))) for i in range(8)]
        for p_ in PS:
            p_.excl = True

        ident = sb(st, "ident", (128, 128), BF16)
        rot = sb(st, "rot", (128, 128), BF16)
        ones = sb(st, "ones", (128, 128), BF16)
        dma("pool", ident[:], k_ident, writes=[ident])
        dma("pool", rot[:], k_rot, writes=[rot])
        op("dve", lambda v: v.memset(ones[:], 1.0), writes=[ones])

        def stiles(with_ctx):
            L = [(t0, 512, False) for t0 in range(0, T, 512)]
            if with_ctx:
                L += [(T + c0, min(512, C - c0), True) for c0 in range(0, C, 512)]
            return L

        with ExitStack() as ph:
            dma("sp", xres[0:T, :], x_in)
            dma("sp", xres[T:NTOK, :], ctx_in)
            cs = sb(ph, "cs", (128, KC, 2), F32)
            csil = sb(ph, "csil", (128, KC, 2), F32)
            dma("sp", cs[:, :, 0], c_in.rearrange("(kc p) -> p kc", p=128), writes=[cs], allow_slow_non_contiguous=True)
            dma("sp", cs[:, :, 1], cctx_in.rearrange("(kc p) -> p kc", p=128), writes=[cs], allow_slow_non_contiguous=True)
            op("act", lambda a: a.activation(out=csil[:], in_=cs[:], func=AF.Silu), reads=[cs], writes=[csil])
            wts = [sb(ph, "adw%d" % i, (128, KC, 512), F32) for i in range(2)]
            bts = [sb(ph, "adb%d" % i, (2, 512), F32) for i in range(2)]
            mts = [sb(ph, "adm%d" % i, (2, 512), F32) for i in range(2)]
            n = 0
            if modv_in:
                dma("sp", modv, modv_src)
            for i in range(0 if modv_in else 2):
                for cb in range(48):
                    w = wts[n % 2]; bt = bts[n % 2]; mt = mts[n % 2]; ps = PS[n % 2]
                    n += 1
                    dma("sp", w[:], ada_w[i].rearrange("(kc p) n -> p kc n", p=128)[:, :, cb * 512:(cb + 1) * 512], writes=[w])
                    for r in range(2):
                        dma("pool", bt[r:r + 1, :], ada_b[i:i + 1, cb * 512:(cb + 1) * 512], writes=[bt])
                    for kc in range(KC):
                        op("pe", lambda t, kc=kc: t.matmul(ps[0:2, :], csil[:, kc, :], w[:, kc, :], start=(kc == 0), stop=(kc == KC - 1)),
                           reads=[csil, w], writes=[ps])
                    op("dve", lambda v: v.tensor_tensor(out=mt[:], in0=ps[0:2, :], in1=bt[:], op=ALU.add), reads=[ps, bt], writes=[mt])
                    dma("sp", modv[i, :, cb * 512:(cb + 1) * 512], mt[:], reads=[mt])
            tr.barrier()

        def load_rep(q, tile, src_row_ap):
            dma(q, tile[:], src_row_ap.partition_broadcast(128), writes=[tile])

        def phase_norm(layer, which, with_ctx):
            with ExitStack() as ph:
                A = [sb(ph, "nA%d" % r, (128, D), F32) for r in range(2)]
                B = [sb(ph, "nB%d" % r, (128, D), F32) for r in range(2)]
                gt = sb(ph, "ng", (128, D), F32)
                load_rep("sp", gt, norm_g[layer, which:which + 1, :])
                for r in range(2 if with_ctx else 1):
                    sh = (0 if which == 0 else 3) * D
                    load_rep("sp", B[r], modv[layer, r:r + 1, sh:sh + D])
                    load_rep("sp", A[r], modv[layer, r:r + 1, sh + D:sh + 2 * D])
                    op("dve", lambda v, r=r: v.scalar_tensor_tensor(out=A[r][:], in0=A[r][:], scalar=1.0, in1=gt[:], op0=ALU.add, op1=ALU.mult),
                       reads=[A[r], gt], writes=[A[r]])
                xts = [sb(ph, "nx%d" % i, (128, D), F32) for i in range(2)]
                sq = sb(ph, "nsq", (128, D), BF16)
                hb = [sb(ph, "nhb%d" % i, (128, D), BF16) for i in range(2)]
                hTt = [sb(ph, "nhT%d" % i, (128, KC, 128), BF16) for i in range(2)]
                ss = [sb(ph, "nss%d" % i, (128, 2), F32) for i in range(2)]
                ntile = (NTOK if with_ctx else T) // 128
                for t in range(ntile):
                    r = 1 if t * 128 >= T else 0
                    xt = xts[t % 2]; h = hb[t % 2]; hT_t = hTt[t % 2]; s_ = ss[t % 2]
                    dma("sp", xt[:], xres[t * 128:(t + 1) * 128, :], writes=[xt])
                    op("dve", lambda v: v.memset(s_[:], 0.0), writes=[s_])
                    op("act", lambda a: a.activation(out=sq[:], in_=xt[:], func=AF.Square, accum_out=s_[:, 0:1]), reads=[xt, s_], writes=[sq, s_])
                    op("dve", lambda v: v.tensor_scalar(out=s_[:, 1:2], in0=s_[:, 0:1], scalar1=1.0 / D, scalar2=EPS, op0=ALU.mult, op1=ALU.add), reads=[s_], writes=[s_])
                    op("act", lambda a: a.activation(out=s_[:, 1:2], in_=s_[:, 1:2], func=AF.Sqrt), reads=[s_], writes=[s_])
                    op("dve", lambda v: v.reciprocal(out=s_[:, 1:2], in_=s_[:, 1:2]), reads=[s_], writes=[s_])
                    op("dve", lambda v: v.scalar_tensor_tensor(out=xt[:], in0=xt[:], scalar=s_[:, 1:2], in1=A[r][:], op0=ALU.mult, op1=ALU.mult),
                       reads=[xt, s_, A[r]], writes=[xt])
                    op("pool", lambda g: g.tensor_tensor(out=h[:], in0=xt[:], in1=B[r][:], op=ALU.add), reads=[xt, B[r]], writes=[h])
                    for q4 in range(4):
                        ps = PS[(t * 4 + q4) % 4]
                        psb = ps[:].bitcast(BF16)
                        for j in range(8):
                            kc = q4 * 8 + j
                            op("pe", lambda te, kc=kc, j=j: te.transpose(psb[:, j * 128:(j + 1) * 128], h[:, kc * 128:(kc + 1) * 128], ident[:]),
                               reads=[h, ident], writes=[ps])
                        eng = "act" if q4 % 2 == 0 else "dve"
                        if eng == "act":
                            op("act", lambda a: a.copy(out=hT_t[:, q4 * 8:(q4 + 1) * 8, :], in_=psb.rearrange("p (j t) -> p j t", j=8)), reads=[ps], writes=[hT_t])
                        else:
                            op("dve", lambda v: v.tensor_copy(out=hT_t[:, q4 * 8:(q4 + 1) * 8, :], in_=psb.rearrange("p (j t) -> p j t", j=8)), reads=[ps], writes=[hT_t])
                    dma("sp", hT.rearrange("(kc p) t -> p kc t", p=128)[:, :, t * 128:(t + 1) * 128], hT_t[:], reads=[hT_t])
                tr.barrier()

        def rope_store(ph_tiles, ps, np_, W, t0, dst_ap):
            zb, t1, t2, ob, cs_t, sn_t, ps2 = ph_tiles
            op("act", lambda a: a.copy(out=zb[0:np_, 0:W], in_=ps[0:np_, 0:W]), reads=[ps], writes=[zb])
            op("pe", lambda te: te.matmul(ps2[0:np_, 0:W], rot[0:np_, 0:np_], zb[0:np_, 0:W], start=True, stop=True), reads=[rot, zb], writes=[ps2])
            op("dve", lambda v: v.tensor_tensor(out=t1[0:np_, 0:W], in0=ps[0:np_, 0:W], in1=cs_t[0:np_, 0:W], op=ALU.mult), reads=[ps, cs_t], writes=[t1])
            op("dve", lambda v: v.tensor_tensor(out=t2[0:np_, 0:W], in0=ps2[0:np_, 0:W], in1=sn_t[0:np_, 0:W], op=ALU.mult), reads=[ps2, sn_t], writes=[t2])
            op("pool", lambda g: g.tensor_tensor(out=ob[0:np_, 0:W], in0=t1[0:np_, 0:W], in1=t2[0:np_, 0:W], op=ALU.add), reads=[t1, t2], writes=[ob])
            dma("sp", dst_ap, ob[0:np_, 0:W], reads=[ob])

        def rope_tiles(ph):
            return [sb(ph, "rzb", (128, 512), BF16), sb(ph, "rt1", (128, 512), F32), sb(ph, "rt2", (128, 512), F32),
                    sb(ph, "rob", (128, 512), BF16), sb(ph, "rcs", (128, 512), F32), sb(ph, "rsn", (128, 512), F32), PS[7]]

        def phase_qkv():
            with ExitStack() as ph:
                hs = [sb(ph, "qh%d" % i, (128, KC, 512), BF16) for i in range(2)]
                ws = [sb(ph, "qw%d" % i, (128, KC, 512), BF16) for i in range(2)]
                rt = rope_tiles(ph)
                ob = sb(ph, "qob", (128, 512), BF16)
                nw = 0
                for si, (t0, W, isc) in enumerate(stiles(True)):
                    h = hs[si % 2]
                    dma("sp", h[:, :, 0:W], hT.rearrange("(kc p) t -> p kc t", p=128)[:, :, t0:t0 + W], writes=[h])
                    if not isc:
                        dma("sp", rt[4][:, 0:W], k_cos[:, t0:t0 + W], writes=[rt[4]])
                        dma("sp", rt[5][:, 0:W], k_sin[:, t0:t0 + W], writes=[rt[5]])
                    for cb in range(10):
                        w = ws[nw % 2]; nw += 1
                        tr.wload("pool", w, a_wqkv.rearrange("(kc p) n -> p kc n", p=128)[:, :, cb * 512:(cb + 1) * 512], KC, key=("qkv", cb))
                        if cb < 9:
                            for m in range(4):
                                ps = PS[m % 2]
                                for kc in range(KC):
                                    op("pe", lambda te, kc=kc: te.matmul(ps[:, 0:W], w[:, kc, m * 128:(m + 1) * 128], h[:, kc, 0:W], start=(kc == 0), stop=(kc == KC - 1)),
                                       reads=[w, h], writes=[ps])
                                row0 = cb * 512 + m * 128
                                dst = (qT[row0:row0 + 128, t0:t0 + W] if cb < 8 else kT[row0 - 4096:row0 - 4096 + 128, t0:t0 + W])
                                QM = int(os.environ.get("QKV_MODE", "9"))
                                if not isc and QM >= 2:
                                    rope_store(rt, ps, 128, W, t0, dst)
                                elif QM >= 1:
                                    op("act", lambda a: a.copy(out=ob[:, 0:W], in_=ps[:, 0:W]), reads=[ps], writes=[ob])
                                    dma("sp", dst, ob[:, 0:W], reads=[ob])
                        elif int(os.environ.get("QKV_MODE", "9")) >= 3:
                            for tt in range(W // 128):
                                ps = PS[2 + tt % 2]
                                for kc in range(KC):
                                    op("pe", lambda te, kc=kc: te.matmul(ps[:], h[:, kc, tt * 128:(tt + 1) * 128], w[:, kc, :], start=(kc == 0), stop=(kc == KC - 1)),
                                       reads=[w, h], writes=[ps])
                                op("act", lambda a: a.copy(out=ob[:], in_=ps[:]), reads=[ps], writes=[ob])
                                dma("sp", vv[t0 + tt * 128:t0 + (tt + 1) * 128, :], ob[:], reads=[ob])
                tr.barrier()

        def phase_att0():
            with ExitStack() as ph:
                mprev = sb(ph, "mprev", (128, 512), BF16)
                mnext = sb(ph, "mnext", (128, 512), BF16)
                dma("pool", mprev[:], k_mprev, writes=[mprev])
                dma("pool", mnext[:], k_mnext, writes=[mnext])
                sk = sb(ph, "sk", (1, 64), F32)
                es1 = sb(ph, "es1", (1, 64), F32)
                esrow = sb(ph, "esrow", (1, 64 * 128), BF16)
                dma("sp", sk[:], a_sink.rearrange("(o h) -> o h", o=1), writes=[sk])
                op("act", lambda a: a.activation(out=es1[:], in_=sk[:], func=AF.Exp), reads=[sk], writes=[es1])
                op("dve", lambda v: v.memset(esrow[:], 0.0), writes=[esrow])
                for hh in range(64):
                    op("dve", lambda v, hh=hh: v.tensor_scalar(out=esrow[:, hh * 128:(hh + 1) * 128], in0=esrow[:, hh * 128:(hh + 1) * 128], scalar1=es1[:, hh:hh + 1], scalar2=None, op0=ALU.add),
                       reads=[esrow, es1], writes=[esrow])
                kc_t = sb(ph, "kct", (64, C), BF16)
                vc_t = sb(ph, "vct", (128, C // 128, 64), BF16)
                qs = [sb(ph, "aq%d" % i, (64, 8, 512), BF16) for i in range(2)]
                ks = [sb(ph, "ak%d" % i, (64, 768), BF16) for i in range(2)]
                vs = [sb(ph, "av%d" % i, (128, 6, 64), BF16) for i in range(2)]
                pts = [sb(ph, "ap%d" % i, (128, 512), BF16) for i in range(3)]
                den = sb(ph, "aden", (64, 512), F32)
                obs = [sb(ph, "aob%d" % i, (64, 512), BF16) for i in range(2)]
                npt = 0
                nob = 0
                nJ = 0
                for g in range(8):
                    dma("sp", kc_t[:], kT[g * 64:(g + 1) * 64, T:T + C], writes=[kc_t])
                    dma("sp", vc_t[:], vv[T:T + C, g * 64:(g + 1) * 64].rearrange("(b p) d -> p b d", p=128), writes=[vc_t])
                    for (t0, W, isc) in stiles(True):
                        q = qs[nJ % 2]; k = ks[nJ % 2]; v = vs[nJ % 2]; nJ += 1
                        dma("sp", q[:, :, 0:W], qT[g * 512:(g + 1) * 512, t0:t0 + W].rearrange("(h d) t -> d h t", d=64), writes=[q])
                        if not isc:
                            k0 = max(t0 - 128, 0); k1 = min(t0 + W + 128, T)
                            koff = k0 - (t0 - 128)
                            dma("sp", k[:, koff:koff + (k1 - k0)], kT[g * 64:(g + 1) * 64, k0:k1], writes=[k])
                            dma("sp", v[:, koff // 128:koff // 128 + (k1 - k0) // 128, :],
                                vv[k0:k1, g * 64:(g + 1) * 64].rearrange("(b p) d -> p b d", p=128), writes=[v])
                        for jb in range(W // 128):
                            tq = t0 + jb * 128
                            kbl = []
                            if not isc:
                                if tq - 128 >= 0:
                                    kbl.append((k[:, jb * 128:(jb + 1) * 128], v[:, jb, :], mprev))
                                kbl.append((k[:, (jb + 1) * 128:(jb + 2) * 128], v[:, jb + 1, :], None))
                                if tq + 256 <= T:
                                    kbl.append((k[:, (jb + 2) * 128:(jb + 3) * 128], v[:, jb + 2, :], mnext))
                            for cbk in range(C // 128):
                                kbl.append((kc_t[:, cbk * 128:(cbk + 1) * 128], vc_t[:, cbk, :], None))
                            for a in range(2):
                                po = PS[4]; pd = PS[5]
                                rhs_q = q[:, a * 4:(a + 1) * 4, jb * 128:(jb + 1) * 128]
                                nkb_ = len(kbl)

                                def qk0(bi):
                                    kap, vap, msk = kbl[bi]
                                    ps = PS[bi % 3]
                                    op("pe", lambda te: te.matmul(ps[:].rearrange("p (h t) -> p h t", h=4), kap, rhs_q, start=True, stop=(msk is None)), reads=[k, kc_t, q], writes=[ps])
                                    if msk is not None:
                                        op("pe", lambda te: te.matmul(ps[:], ident[:], msk[:], start=False, stop=True), reads=[ident, msk], writes=[ps])

                                def rest0(bi, pt):
                                    kap, vap, msk = kbl[bi]
                                    ps = PS[bi % 3]
                                    op("act", lambda ac: ac.activation(out=pt[:], in_=ps[:], func=AF.Exp, scale=0.125), reads=[ps], writes=[pt])
                                    op("pe", lambda te: te.matmul(po[0:64, :], vap, pt[:], start=(bi == 0), stop=(bi == nkb_ - 1)), reads=[v, vc_t, pt], writes=[po])
                                    op("pe", lambda te: te.matmul(pd[0:64, :], ones[:, 0:64], pt[:], start=(bi == 0), stop=False), reads=[ones, pt], writes=[pd])

                                for bi in range(min(2, nkb_)):
                                    qk0(bi)
                                for bi in range(nkb_):
                                    if bi + 2 < nkb_:
                                        qk0(bi + 2)
                                    rest0(bi, pts[npt % 3])
                                    npt += 1
                                op("pe", lambda te: te.matmul(pd[0:64, :], ones[0:1, 0:64], esrow[0:1, (g * 8 + a * 4) * 128:(g * 8 + a * 4 + 4) * 128], start=False, stop=True),
                                   reads=[ones, esrow], writes=[pd])
                                ob = obs[nob % 2]; nob += 1
                                op("dve", lambda vE: vE.reciprocal(out=den[:], in_=pd[0:64, :]), reads=[pd], writes=[den])
                                op("dve", lambda vE: vE.tensor_tensor(out=ob[:], in0=po[0:64, :], in1=den[:], op=ALU.mult), reads=[po, den], writes=[ob])
                                h0 = g * 8 + a * 4
                                dma("sp", oT[h0 * 64:(h0 + 4) * 64, tq:tq + 128].rearrange("(h d) t -> d h t", d=64),
                                    ob[:].rearrange("d (h t) -> d h t", h=4), reads=[ob])
                tr.barrier()

        def phase_wo(layer, wo, with_ctx):
            with ExitStack() as ph:
                G = [sb(ph, "wG%d" % r, (128, D), F32) for r in range(2)]
                for r in range(2 if with_ctx else 1):
                    load_rep("sp", G[r], modv[layer, r:r + 1, 2 * D:3 * D])
                os_ = [sb(ph, "wo_o%d" % i, (128, KC, 512), BF16) for i in range(2)]
                ws = [sb(ph, "wo_w%d" % i, (128, KC, 512), BF16) for i in range(2)]
                xb = [sb(ph, "wo_x%d" % i, (128, 512), F32) for i in range(3)]
                tb = [sb(ph, "wo_t%d" % i, (128, 512), F32) for i in range(3)]
                nw = 0; nx = 0
                for si, (t0, W, isc) in enumerate(stiles(with_ctx)):
                    o = os_[si % 2]
                    r = 1 if isc else 0
                    dma("sp", o[:, :, 0:W], oT.rearrange("(kc p) t -> p kc t", p=128)[:, :, t0:t0 + W], writes=[o])
                    for cb in range(8):
                        w = ws[nw % 2]; nw += 1
                        tr.wload("pool", w, wo.rearrange("(kc p) n -> p kc n", p=128)[:, :, cb * 512:(cb + 1) * 512], KC, key=("wo", layer, cb))
                        for tt in range(W // 128):
                            ps = PS[tt % 4]
                            x_ = xb[nx % 3]; t_ = tb[nx % 3]; nx += 1
                            rows = slice(t0 + tt * 128, t0 + (tt + 1) * 128)
                            dma("sp", x_[:], xres[rows, cb * 512:(cb + 1) * 512], writes=[x_])
                            for kc in range(KC):
                                op("pe", lambda te, kc=kc: te.matmul(ps[:], o[:, kc, tt * 128:(tt + 1) * 128], w[:, kc, :], start=(kc == 0), stop=(kc == KC - 1)),
                                   reads=[o, w], writes=[ps])
                            op("dve", lambda v: v.tensor_tensor(out=t_[:], in0=ps[:], in1=G[r][:, cb * 512:(cb + 1) * 512], op=ALU.mult), reads=[ps, G[r]], writes=[t_])
                            op("pool", lambda g_: g_.tensor_tensor(out=t_[:], in0=t_[:], in1=x_[:], op=ALU.add), reads=[t_, x_], writes=[t_])
                            dma("sp", xres[rows, cb * 512:(cb + 1) * 512], t_[:], reads=[t_])
                tr.barrier()

        def phase_moe(layer, with_ctx):
            with ExitStack() as ph:
                G = [sb(ph, "mG%d" % r, (128, D), F32) for r in range(2)]
                for r in range(2 if with_ctx else 1):
                    load_rep("sp", G[r], modv[layer, r:r + 1, 5 * D:6 * D])
                wr = sb(ph, "m_wr", (128, KC, NR), BF16)
                rb = sb(ph, "m_rb", (1, NR), BF16)
                tr.wload("pool", wr, r_w[layer].rearrange("(kc p) n -> p kc n", p=128), KC)
                dma("pool", rb[:], r_b[layer:layer + 1, :], writes=[rb])
                hs = sb(ph, "m_h", (128, KC, 512), BF16)
                wg = [sb(ph, "m_wg%d" % i, (128, KC, 256), BF16) for i in range(2)]
                wd = [sb(ph, "m_wd%d" % i, (128, 4, 1024), BF16) for i in range(2)]
                yacc = sb(ph, "m_y", (128, 4, D), F32)
                sg = sb(ph, "m_sg", (128, 2, 512), F32)
                actT = sb(ph, "m_act", (128, 4, 512), BF16)
                gate = sb(ph, "m_gate", (128, 4, NE), F32)
                lg = sb(ph, "m_lg", (128, NR), F32)
                sm = sb(ph, "m_sm", (128, 8 + 4 * NG), F32)
                o1, o2, o3, o4 = 8, 8 + NG, 8 + 2 * NG, 8 + 3 * NG
                t8 = sb(ph, "m_t8", (128, NG), F32)
                m8 = sb(ph, "m_m8", (128, NG), F32)
                e32 = sb(ph, "m_e32", (128, NE), F32)
                k32 = sb(ph, "m_k32", (128, NE), F32)
                xb = [sb(ph, "m_x%d" % i, (128, 512), F32) for i in range(2)]
                tb = [sb(ph, "m_t%d" % i, (128, 512), F32) for i in range(2)]
                nwg = 0; nwd = 0; nx = 0
                for si, (t0, W, isc) in enumerate(stiles(with_ctx)):
                    r = 1 if isc else 0
                    ntt = W // 128
                    dma("sp", hs[:, :, 0:W], hT.rearrange("(kc p) t -> p kc t", p=128)[:, :, t0:t0 + W], writes=[hs])
                    for tt in range(ntt):
                        ps = PS[6]
                        for kc in range(KC):
                            op("pe", lambda te, kc=kc: te.matmul(ps[:, 0:NR], hs[:, kc, tt * 128:(tt + 1) * 128], wr[:, kc, :], start=(kc == 0), stop=False), reads=[hs, wr], writes=[ps])
                        op("pe", lambda te: te.matmul(ps[:, 0:NR], ones[0:1, :], rb[:], start=False, stop=True), reads=[ones, rb], writes=[ps])
                        op("dve", lambda v: v.tensor_copy(out=lg[:], in_=ps[:, 0:NR]), reads=[ps], writes=[lg])
                        op("dve", lambda v: v.tensor_reduce(out=sm[:, 0:1], in_=lg[:, 0:NG], axis=AX.X, op=ALU.max), reads=[lg], writes=[sm])
                        op("dve", lambda v: v.tensor_scalar(out=m8[:], in0=lg[:, 0:NG], scalar1=sm[:, 0:1], scalar2=None, op0=ALU.is_ge), reads=[lg, sm], writes=[m8])
                        op("dve", lambda v: v.tensor_scalar(out=t8[:], in0=lg[:, 0:NG], scalar1=sm[:, 0:1], scalar2=None, op0=ALU.subtract), reads=[lg, sm], writes=[t8])
                        op("dve", lambda v: v.memset(sm[:, 1:2], 0.0), writes=[sm])
                        op("act", lambda a: a.activation(out=t8[:], in_=t8[:], func=AF.Exp, accum_out=sm[:, 1:2]), reads=[t8, sm], writes=[t8, sm])
                        op("dve", lambda v: v.reciprocal(out=sm[:, 2:3], in_=sm[:, 1:2]), reads=[sm], writes=[sm])
                        le = lg[:, NG:NR].rearrange("p (g e) -> p g e", e=4)
                        op("dve", lambda v: v.tensor_reduce(out=sm[:, o1:o2], in_=le, axis=AX.X, op=ALU.max), reads=[lg], writes=[sm])
                        mx1 = sm[:, o1:o2].unsqueeze(2).to_broadcast([128, NG, 4])
                        k3 = k32[:].rearrange("p (g e) -> p g e", e=4)
                        e3 = e32[:].rearrange("p (g e) -> p g e", e=4)
                        op("dve", lambda v: v.tensor_tensor(out=k3, in0=le, in1=mx1, op=ALU.is_ge), reads=[lg, sm], writes=[k32])
                        op("dve", lambda v: v.tensor_tensor(out=e3, in0=le, in1=mx1, op=ALU.subtract), reads=[lg, sm], writes=[e32])
                        op("dve", lambda v: v.scalar_tensor_tensor(out=k32[:], in0=k32[:], scalar=-1e9, in1=e32[:], op0=ALU.mult, op1=ALU.add), reads=[k32, e32], writes=[k32])
                        op("dve", lambda v: v.tensor_reduce(out=sm[:, o2:o3], in_=k3, axis=AX.X, op=ALU.max), reads=[k32], writes=[sm])
                        mx2 = sm[:, o2:o3].unsqueeze(2).to_broadcast([128, NG, 4])
                        op("dve", lambda v: v.tensor_tensor(out=k3, in0=e3, in1=mx2, op=ALU.is_ge), reads=[e32, sm], writes=[k32])
                        op("act", lambda a: a.activation(out=e32[:], in_=e32[:], func=AF.Exp), reads=[e32], writes=[e32])
                        op("dve", lambda v: v.tensor_tensor(out=e32[:], in0=e32[:], in1=k32[:], op=ALU.mult), reads=[e32, k32], writes=[e32])
                        op("dve", lambda v: v.tensor_reduce(out=sm[:, o3:o4], in_=e3, axis=AX.X, op=ALU.add), reads=[e32], writes=[sm])
                        op("dve", lambda v: v.reciprocal(out=sm[:, o4:o4 + NG], in_=sm[:, o3:o4]), reads=[sm], writes=[sm])
                        op("dve", lambda v: v.tensor_tensor(out=m8[:], in0=m8[:], in1=sm[:, o4:o4 + NG], op=ALU.mult), reads=[m8, sm], writes=[m8])
                        op("dve", lambda v: v.tensor_scalar(out=m8[:], in0=m8[:], scalar1=sm[:, 2:3], scalar2=None, op0=ALU.mult), reads=[m8, sm], writes=[m8])
                        op("dve", lambda v: v.tensor_tensor(out=gate[:, tt, :].rearrange("p (g e) -> p g e", e=4), in0=e3, in1=m8[:].unsqueeze(2).to_broadcast([128, NG, 4]), op=ALU.mult),
                           reads=[e32, m8], writes=[gate])
                    for e in range(NE):
                        for half in range(2):
                            for gu in range(2):
                                w = wg[nwg % 2]; nwg += 1
                                c0 = gu * 512 + half * 256
                                tr.wload("pool", w, e_wgu[layer, e].rearrange("(kc p) n -> p kc n", p=128)[:, :, c0:c0 + 256], KC, key=("gu", layer, e, c0))
                                for m in range(2):
                                    ps = PS[(gu * 2 + m) % 4]
                                    for kc in range(KC):
                                        op("pe", lambda te, kc=kc: te.matmul(ps[:, 0:W], w[:, kc, m * 128:(m + 1) * 128], hs[:, kc, 0:W], start=(kc == 0), stop=(kc == KC - 1)),
                                           reads=[w, hs], writes=[ps])
                                    if gu == 0:
                                        op("act", lambda a: a.activation(out=sg[:, m, 0:W], in_=ps[:, 0:W], func=AF.Silu), reads=[ps], writes=[sg])
                                    else:
                                        op("dve", lambda v: v.tensor_tensor(out=actT[:, half * 2 + m, 0:W], in0=ps[:, 0:W], in1=sg[:, m, 0:W], op=ALU.mult), reads=[ps, sg], writes=[actT])
                        for dh in range(4):
                            w = wd[nwd % 2]; nwd += 1
                            tr.wload("pool", w, e_wdn[layer, e].rearrange("(kc p) n -> p kc n", p=128)[:, :, dh * 1024:(dh + 1) * 1024], 4, key=("dn", layer, e, dh))
                            for tt in range(ntt):
                                for cbl in range(2):
                                    cb = dh * 2 + cbl
                                    ps = PS[4 + (tt * 2 + cbl) % 2]
                                    for kc in range(4):
                                        op("pe", lambda te, kc=kc: te.matmul(ps[:], actT[:, kc, tt * 128:(tt + 1) * 128], w[:, kc, cbl * 512:(cbl + 1) * 512], start=(kc == 0), stop=(kc == 3)),
                                           reads=[actT, w], writes=[ps])
                                    ysl = yacc[:, tt, cb * 512:(cb + 1) * 512]
                                    if e == 0:
                                        op("dve", lambda v: v.tensor_scalar(out=ysl, in0=ps[:], scalar1=gate[:, tt, e:e + 1], scalar2=None, op0=ALU.mult), reads=[ps, gate], writes=[yacc])
                                    else:
                                        op("dve", lambda v: v.scalar_tensor_tensor(out=ysl, in0=ps[:], scalar=gate[:, tt, e:e + 1], in1=ysl, op0=ALU.mult, op1=ALU.add), reads=[ps, gate, yacc], writes=[yacc])
                    for tt in range(ntt):
                        rows = slice(t0 + tt * 128, t0 + (tt + 1) * 128)
                        for cb in range(8):
                            x_ = xb[nx % 2]; t_ = tb[nx % 2]; nx += 1
                            dma("sp", x_[:], xres[rows, cb * 512:(cb + 1) * 512], writes=[x_])
                            op("pool", lambda g_: g_.tensor_tensor(out=t_[:], in0=yacc[:, tt, cb * 512:(cb + 1) * 512], in1=G[r][:, cb * 512:(cb + 1) * 512], op=ALU.mult), reads=[yacc, G[r]], writes=[t_])
                            op("pool", lambda g_: g_.tensor_tensor(out=t_[:], in0=t_[:], in1=x_[:], op=ALU.add), reads=[t_, x_], writes=[t_])
                            dma("sp", xres[rows, cb * 512:(cb + 1) * 512], t_[:], reads=[t_])
                tr.barrier()

        def phase_dkv():
            with ExitStack() as ph:
                hs = [sb(ph, "dh%d" % i, (128, KC, 512), BF16) for i in range(2)]
                ws = [sb(ph, "dw%d" % i, (128, KC, 512), BF16) for i in range(2)]
                rt = rope_tiles(ph)
                zf = sb(ph, "dzf", (128, 12, 512), F32)
                sqb = [sb(ph, "dsq%d" % i, (128, 512), BF16) for i in range(2)]
                rstd = sb(ph, "drs", (128, 512), F32)
                gq = sb(ph, "dgq", (128, 8), F32)
                gkv = sb(ph, "dgkv", (128, 4), F32)
                ob = [sb(ph, "dob%d" % i, (128, 512), BF16) for i in range(2)]
                dma("sp", gq[:], b_gq.rearrange("(c p) -> p c", p=128), writes=[gq], allow_slow_non_contiguous=True)
                dma("sp", gkv[:], b_gkv.rearrange("(c p) -> p c", p=128), writes=[gkv], allow_slow_non_contiguous=True)
                nw = 0; nsq = 0; nob = 0
                for si, (t0, W, isc) in enumerate(stiles(True)):
                    h = hs[si % 2]
                    dma("sp", h[:, :, 0:W], hT.rearrange("(kc p) t -> p kc t", p=128)[:, :, t0:t0 + W], writes=[h])
                    if not isc:
                        dma("sp", rt[4][:, 0:W], k_cos[:, t0:t0 + W], writes=[rt[4]])
                        dma("sp", rt[5][:, 0:W], k_sin[:, t0:t0 + W], writes=[rt[5]])
                    for cb in range(4):
                        if cb < 2 and isc:
                            continue
                        w = ws[nw % 2]; nw += 1
                        ncol = 512 if cb < 3 else 64
                        tr.wload("pool", (w if ncol == 512 else Tview(w, ncol)), b_wdkv.rearrange("(kc p) n -> p kc n", p=128)[:, :, cb * 512:cb * 512 + ncol], KC, key=(("dkv", cb) if ncol == 512 else None))
                        for m in range(4 if cb < 3 else 1):
                            mw = 128 if cb < 3 else 64
                            ps = PS[m % 2]
                            for kc in range(KC):
                                op("pe", lambda te, kc=kc: te.matmul(ps[0:mw, 0:W], w[:, kc, m * 128:m * 128 + mw], h[:, kc, 0:W], start=(kc == 0), stop=(kc == KC - 1)),
                                   reads=[w, h], writes=[ps])
                            if cb < 3:
                                ci = cb * 4 + m
                                sq_ = sqb[nsq % 2]; nsq += 1
                                first = (ci in (0, 8)); last = (ci in (7, 11))
                                pss = PS[2] if ci < 8 else PS[3]
                                op("act", lambda a: a.activation(out=sq_[:, 0:W], in_=ps[:, 0:W], func=AF.Square), reads=[ps], writes=[sq_])
                                op("dve", lambda v: v.tensor_copy(out=zf[:, ci, 0:W], in_=ps[:, 0:W]), reads=[ps], writes=[zf])
                                op("pe", lambda te: te.matmul(pss[:, 0:W], ones[:], sq_[:, 0:W], start=first, stop=last), reads=[ones, sq_], writes=[pss])
                                if last:
                                    n_ = 1024.0 if ci == 7 else 512.0
                                    op("dve", lambda v: v.tensor_scalar(out=rstd[:, 0:W], in0=pss[:, 0:W], scalar1=1.0 / n_, scalar2=EPS, op0=ALU.mult, op1=ALU.add), reads=[pss], writes=[rstd])
                                    op("act", lambda a: a.activation(out=rstd[:, 0:W], in_=rstd[:, 0:W], func=AF.Sqrt), reads=[rstd], writes=[rstd])
                                    op("dve", lambda v: v.reciprocal(out=rstd[:, 0:W], in_=rstd[:, 0:W]), reads=[rstd], writes=[rstd])
                                    for c2 in (range(8) if ci == 7 else range(8, 12)):
                                        o_ = ob[nob % 2]; nob += 1
                                        gcol = gq[:, c2:c2 + 1] if c2 < 8 else gkv[:, c2 - 8:c2 - 7]
                                        op("dve", lambda v: v.scalar_tensor_tensor(out=o_[:, 0:W], in0=zf[:, c2, 0:W], scalar=gcol, in1=rstd[:, 0:W], op0=ALU.mult, op1=ALU.mult),
                                           reads=[zf, gq, gkv, rstd], writes=[o_])
                                        dst = zqT[c2 * 128:(c2 + 1) * 128, t0:t0 + W] if c2 < 8 else ckvT[(c2 - 8) * 128:(c2 - 7) * 128, t0:t0 + W]
                                        dma("sp", dst, o_[:, 0:W], reads=[o_])
                            else:
                                if not isc:
                                    rope_store(rt, ps, 64, W, t0, krT[:, t0:t0 + W])
                                else:
                                    o_ = ob[nob % 2]; nob += 1
                                    op("act", lambda a: a.copy(out=o_[0:64, 0:W], in_=ps[0:64, 0:W]), reads=[ps], writes=[o_])
                                    dma("sp", krT[:, t0:t0 + W], o_[0:64, 0:W], reads=[o_])
                tr.barrier()

        def phase_uq():
            with ExitStack() as ph:
                zs = [sb(ph, "uz%d" % i, (128, 8, 512), BF16) for i in range(2)]
                ws = [sb(ph, "uw%d" % i, (128, 8, 768), BF16) for i in range(2)]
                rt = rope_tiles(ph)
                ob = [sb(ph, "uob%d" % i, (128, 512), BF16) for i in range(2)]
                nw = 0; nob = 0
                for si, (t0, W, isc) in enumerate(stiles(False)):
                    z = zs[si % 2]
                    dma("sp", z[:, :, 0:W], zqT.rearrange("(kc p) t -> p kc t", p=128)[:, :, t0:t0 + W], writes=[z])
                    dma("sp", rt[4][:, 0:W], k_cos[:, t0:t0 + W], writes=[rt[4]])
                    dma("sp", rt[5][:, 0:W], k_sin[:, t0:t0 + W], writes=[rt[5]])
                    for hg in range(8):
                        w = ws[nw % 2]; nw += 1
                        tr.wload("pool", w, b_wuq.rearrange("(kc p) n -> p kc n", p=128)[:, :, hg * 768:(hg + 1) * 768], 8, key=("uq", hg))
                        for hl in range(4):
                            hd = hg * 4 + hl
                            ps = PS[hl % 2]
                            for kc in range(8):
                                op("pe", lambda te, kc=kc: te.matmul(ps[:, 0:W], w[:, kc, hl * 192:hl * 192 + 128], z[:, kc, 0:W], start=(kc == 0), stop=(kc == 7)), reads=[w, z], writes=[ps])
                            o_ = ob[nob % 2]; nob += 1
                            op("act", lambda a: a.copy(out=o_[:, 0:W], in_=ps[:, 0:W]), reads=[ps], writes=[o_])
                            dma("sp", qnT[hd * 128:(hd + 1) * 128, t0:t0 + W], o_[:, 0:W], reads=[o_])
                            ps = PS[2 + hl % 2]
                            for kc in range(8):
                                op("pe", lambda te, kc=kc: te.matmul(ps[0:64, 0:W], w[:, kc, hl * 192 + 128:hl * 192 + 192], z[:, kc, 0:W], start=(kc == 0), stop=(kc == 7)), reads=[w, z], writes=[ps])
                            rope_store(rt, ps, 64, W, t0, qrT[hd * 64:(hd + 1) * 64, t0:t0 + W])
                tr.barrier()

        def phase_mla():
            NKB = NTOK // 128
            sc = (128 + 64) ** -0.5
            with ExitStack() as ph:
                ckv = sb(ph, "l_ckv", (128, 4, NTOK), BF16)
                kr = sb(ph, "l_kr", (64, NTOK), BF16)
                dma("sp", ckv[:], ckvT.rearrange("(kc p) t -> p kc t", p=128), writes=[ckv])
                dma("sp", kr[:], krT, writes=[kr])
                wk = [sb(ph, "l_wk%d" % i, (128, 4, 256), BF16) for i in range(2)]
                kn = sb(ph, "l_kn", (128, NTOK), BF16)
                vh = sb(ph, "l_vh", (128, NKB, 128), BF16)
                qn = [sb(ph, "l_qn%d" % i, (128, 512), BF16) for i in range(2)]
                qr = [sb(ph, "l_qr%d" % i, (64, 512), BF16) for i in range(2)]
                pts = [sb(ph, "l_p%d" % i, (128, 512), BF16) for i in range(3)]
                den = sb(ph, "l_den", (128, 512), F32)
                obs = [sb(ph, "l_ob%d" % i, (128, 512), BF16) for i in range(2)]
                nq = 0; npt = 0; nob = 0; nps = 0
                for hd in range(32):
                    w = wk[hd % 2]
                    tr.wload("pool", w, b_wukv.rearrange("(kc p) n -> p kc n", p=128)[:, :, hd * 256:(hd + 1) * 256], 4)
                    for k0 in range(0, NTOK, 512):
                        kw = min(512, NTOK - k0)
                        ps = PS[nps % 2]; nps += 1
                        for kc in range(4):
                            op("pe", lambda te, kc=kc: te.matmul(ps[:, 0:kw], w[:, kc, 0:128], ckv[:, kc, k0:k0 + kw], start=(kc == 0), stop=(kc == 3)), reads=[w, ckv], writes=[ps])
                        op("dve", lambda v: v.tensor_copy(out=kn[:, k0:k0 + kw], in_=ps[:, 0:kw]), reads=[ps], writes=[kn])
                    for kb0 in range(0, NKB, 4):
                        nb = min(4, NKB - kb0)
                        ps = PS[nps % 2]; nps += 1
                        for bi in range(nb):
                            kb = kb0 + bi
                            for kc in range(4):
                                op("pe", lambda te, kc=kc: te.matmul(ps[:, bi * 128:(bi + 1) * 128], ckv[:, kc, kb * 128:(kb + 1) * 128], w[:, kc, 128:256], start=(kc == 0), stop=(kc == 3)), reads=[w, ckv], writes=[ps])
                        op("act", lambda a: a.copy(out=vh[:, kb0:kb0 + nb, :], in_=ps[:, 0:nb * 128].rearrange("p (b d) -> p b d", d=128)), reads=[ps], writes=[vh])
                    for (t0, W, isc) in stiles(False):
                        qn_ = qn[nq % 2]; qr_ = qr[nq % 2]; nq += 1
                        dma("pool", qn_[:, 0:W], qnT[hd * 128:(hd + 1) * 128, t0:t0 + W], writes=[qn_])
                        dma("pool", qr_[:, 0:W], qrT[hd * 64:(hd + 1) * 64, t0:t0 + W], writes=[qr_])
                        po = PS[5]; pd = PS[6]
                        def qk(kb):
                            ps = PS[2 + kb % 3]
                            op("pe", lambda te: te.matmul(ps[:, 0:W], kn[:, kb * 128:(kb + 1) * 128], qn_[:, 0:W], start=True, stop=False), reads=[kn, qn_], writes=[ps])
                            op("pe", lambda te: te.matmul(ps[:, 0:W], kr[:, kb * 128:(kb + 1) * 128], qr_[:, 0:W], start=False, stop=True), reads=[kr, qr_], writes=[ps])

                        def rest(kb, pt):
                            ps = PS[2 + kb % 3]
                            op("act", lambda a: a.activation(out=pt[:, 0:W], in_=ps[:, 0:W], func=AF.Exp, scale=sc), reads=[ps], writes=[pt])
                            op("pe", lambda te: te.matmul(po[:, 0:W], vh[:, kb, :], pt[:, 0:W], start=(kb == 0), stop=(kb == NKB - 1)), reads=[vh, pt], writes=[po])
                            op("pe", lambda te: te.matmul(pd[:, 0:W], ones[:], pt[:, 0:W], start=(kb == 0), stop=(kb == NKB - 1)), reads=[ones, pt], writes=[pd])

                        for kb in range(min(2, NKB)):
                            qk(kb)
                        for kb in range(NKB):
                            if kb + 2 < NKB:
                                qk(kb + 2)
                            rest(kb, pts[npt % 3])
                            npt += 1
                        ob = obs[nob % 2]; nob += 1
                        op("dve", lambda v: v.reciprocal(out=den[:, 0:W], in_=pd[:, 0:W]), reads=[pd], writes=[den])
                        op("dve", lambda v: v.tensor_tensor(out=ob[:, 0:W], in0=po[:, 0:W], in1=den[:, 0:W], op=ALU.mult), reads=[po, den], writes=[ob])
                        dma("sp", oT[hd * 128:(hd + 1) * 128, t0:t0 + W], ob[:, 0:W], reads=[ob])
                tr.barrier()

        def phase_final():
            with ExitStack() as ph:
                gt = sb(ph, "fg", (128, D), F32)
                load_rep("sp", gt, final_g.rearrange("(o d) -> o d", o=1))
                xts = [sb(ph, "fx%d" % i, (128, D), F32) for i in range(2)]
                sq = sb(ph, "fsq", (128, D), BF16)
                ss = [sb(ph, "fss%d" % i, (128, 2), F32) for i in range(2)]
                for t in range(T // 128):
                    xt = xts[t % 2]; s_ = ss[t % 2]
                    dma("sp", xt[:], xres[t * 128:(t + 1) * 128, :], writes=[xt])
                    op("dve", lambda v: v.memset(s_[:], 0.0), writes=[s_])
                    op("act", lambda a: a.activation(out=sq[:], in_=xt[:], func=AF.Square, accum_out=s_[:, 0:1]), reads=[xt, s_], writes=[sq, s_])
                    op("dve", lambda v: v.tensor_scalar(out=s_[:, 1:2], in0=s_[:, 0:1], scalar1=1.0 / D, scalar2=EPS, op0=ALU.mult, op1=ALU.add), reads=[s_], writes=[s_])
                    op("act", lambda a: a.activation(out=s_[:, 1:2], in_=s_[:, 1:2], func=AF.Sqrt), reads=[s_], writes=[s_])
                    op("dve", lambda v: v.reciprocal(out=s_[:, 1:2], in_=s_[:, 1:2]), reads=[s_], writes=[s_])
                    op("dve", lambda v: v.scalar_tensor_tensor(out=xt[:], in0=xt[:], scalar=s_[:, 1:2], in1=gt[:], op0=ALU.mult, op1=ALU.mult), reads=[xt, s_, gt], writes=[xt])
                    dma("sp", out[t * 128:(t + 1) * 128, :], xt[:], reads=[xt])
                tr.barrier()

        prog = [lambda: phase_norm(0, 0, True), phase_qkv, phase_att0, lambda: phase_wo(0, a_wo, True),
                lambda: phase_norm(0, 1, True), lambda: phase_moe(0, True), lambda: phase_norm(1, 0, True),
                phase_dkv, phase_uq, phase_mla, lambda: phase_wo(1, b_wo, False), lambda: phase_norm(1, 1, False),
                lambda: phase_moe(1, False), phase_final]
        for f in prog[:upto]:
            f()
    return nc


def host_consts(T, GRID_W=64):
    rows = T // GRID_W
    row = np.broadcast_to(np.arange(rows, dtype=np.float32)[:, None], (rows, GRID_W)).reshape(-1)
    col = np.broadcast_to(np.arange(GRID_W, dtype=np.float32)[None, :], (rows, GRID_W)).reshape(-1)
    n_freq = 16
    inv = (np.float32(10000.0) ** (-np.arange(n_freq, dtype=np.float32) / np.float32(n_freq))).astype(np.float32)
    ang = np.concatenate([row[:, None] * inv, col[:, None] * inv], axis=-1).astype(np.float32)
    cos = np.cos(ang).astype(np.float32).T
    sin = np.sin(ang).astype(np.float32).T
    k_cos = np.ascontiguousarray(np.tile(cos, (4, 1)))
    k_sin = np.ascontiguousarray(np.tile(sin, (4, 1)))
    rotT = np.zeros((128, 128), np.float32)
    for blk in (0, 64):
        for m in range(32):
            rotT[blk + m + 32, blk + m] = -1.0
            rotT[blk + m, blk + m + 32] = 1.0
    p = np.arange(128)[:, None]
    i = np.arange(128)[None, :]
    mprev = np.where(p >= i, 0.0, NEGM).astype(np.float32)
    mnext = np.where(p <= i, 0.0, NEGM).astype(np.float32)
    return dict(k_ident=np.eye(128, dtype=np.float32), k_rot=rotT, k_cos=k_cos, k_sin=k_sin,
                k_mprev=np.ascontiguousarray(np.tile(mprev, (1, 4))), k_mnext=np.ascontiguousarray(np.tile(mnext, (1, 4))))


def make_in_maps(inp, T, C):
    B = inp["x"].shape[0]
    hc = host_consts(T)
    r_w = np.ascontiguousarray(np.concatenate([inp["r_wg"], inp["r_we"]], axis=-1))
    r_b = np.ascontiguousarray(np.concatenate([inp["r_bg"], inp["r_be"]], axis=-1))
    maps = []
    for b in range(B):
        m = dict(x=inp["x"][b], c=inp["c"][b], ctx=inp["ctx"][b], c_ctx=inp["c_ctx"], ada_w=inp["ada_w"], ada_b=inp["ada_b"],
                 norm_g=inp["norm_g"], final_g=inp["final_g"], a_wqkv=inp["a_wqkv"][0], a_wo=inp["a_wo"][0], a_sink=inp["a_sink"][0],
                 b_wdkv=inp["b_wdkv"][0], b_gq=inp["b_gq"][0], b_gkv=inp["b_gkv"][0], b_wuq=inp["b_wuq"][0], b_wukv=inp["b_wukv"][0],
                 b_wo=inp["b_wo"][0], r_w=r_w, r_b=r_b, e_wgu=inp["e_wgu"], e_wdn=inp["e_wdn"])
        m.update(hc)
        maps.append({k: np.ascontiguousarray(np.asarray(v, dtype=np.float32)) for k, v in m.items()})
    return maps


def kernel(**inputs):
    inp = {k: np.asarray(v) for k, v in inputs.items()}
    B, T, _ = inp["x"].shape
    C = inp["ctx"].shape[1]
    NE = inp["e_wgu"].shape[1]
    nc = build(T, C, NE)
    maps = make_in_maps(inp, T, C)
    res = run_bass_kernel_spmd(nc, maps, core_ids=list(range(B)))
    return np.stack([res.results[b]["out"] for b in range(B)], axis=0).astype(np.float32)
```

```python
import os
import numpy as np
from contextlib import ExitStack
import concourse.bass as bass
import concourse.mybir as mybir
from concourse.bass_utils import run_bass_kernel_spmd

F32 = mybir.dt.float32
BF16 = mybir.dt.bfloat16
AF = mybir.ActivationFunctionType
ALU = mybir.AluOpType
AX = mybir.AxisListType

D = 4096
KC = 32
EPS = 1e-6
NEGM = -30000.0


class Res:
    def __init__(self):
        self.w = None
        self.r = {}


class Tile(Res):
    def __init__(self, t):
        super().__init__()
        self.t = t

    def __getitem__(self, k):
        return self.t[k]


class Tview:
    def __init__(self, tile, ncol):
        self.tile = tile
        self.ncol = ncol

    def __getitem__(self, k):
        return self.tile.t[k[0], k[1], 0:self.ncol]


class Tr:
    def __init__(self, nc, st):
        self.nc = nc
        self.wcache = {}
        self.sems = []

        def mk(name):
            self.sems.append(st.enter_context(nc.semaphore(name)))
            return len(self.sems) - 1

        self.E = {}
        for nm, eng in (("pe", nc.tensor), ("act", nc.scalar), ("dve", nc.vector),
                        ("pool", nc.gpsimd), ("sp", nc.sync)):
            self.E[nm] = dict(eng=eng, sem=mk("s_" + nm), cnt=0, known={})
        self.dq = {}
        for q, n in (("sp", 8), ("pool", 8), ("act", 4)):
            self.dq[q] = dict(sl=[[mk("d_%s%d" % (q, i)), 0] for i in range(n)], i=0)

    def _wait(self, e, toks):
        E = self.E[e]
        for (sm, v) in toks:
            if E["known"].get(sm, 0) < v:
                E["eng"].wait_ge(self.sems[sm], v)
                E["known"][sm] = v

    def _deps(self, e, reads, writes):
        toks = []
        for r in reads:
            if r.w:
                toks.append(r.w)
        for w in writes:
            if w.w:
                toks.append(w.w)
            toks.extend(w.r.items())
        if e == "pe":
            ps = self.E["pe"]["sem"]
            toks = [t for t in toks if t[0] != ps]
        return toks

    def _mark(self, tok, reads, writes):
        for r in reads:
            if r.r.get(tok[0], 0) < tok[1]:
                r.r[tok[0]] = tok[1]
        for w in writes:
            w.w = tok
            w.r = {}

    def op(self, e, fn, reads=(), writes=()):
        writes = list(writes) + [r for r in reads if getattr(r, "excl", False) and r not in writes]
        self._wait(e, self._deps(e, reads, writes))
        E = self.E[e]
        ins = fn(E["eng"])
        E["cnt"] += 1
        ins.then_inc(self.sems[E["sem"]], 1)
        self._mark((E["sem"], E["cnt"]), reads, writes)

    def dma(self, q, out, in_, reads=(), writes=(), **kw):
        Q = self.dq[q]
        sl = Q["sl"][Q["i"] % len(Q["sl"])]
        Q["i"] += 1
        toks = self._deps(q, reads, writes) + [(sl[0], sl[1])]
        self._wait(q, toks)
        self.E[q]["eng"].dma_start(out=out, in_=in_, **kw).then_inc(self.sems[sl[0]], 16)
        sl[1] += 16
        self._mark((sl[0], sl[1]), reads, writes)

    def wload(self, q, tile, src, n1, step=4, key=None):
        if isinstance(tile, Tview) or key is None:
            res = tile.tile if isinstance(tile, Tview) else tile
            self.dma(q, tile[:, 0:n1], src[:, 0:n1], writes=[res])
            return
        flat = tile.t[:].rearrange("p a b -> p (a b)")
        if key in self.wcache:
            cap, cres = self.wcache[key]
            self.dma(q, flat, cap, reads=[cres], writes=[tile])
            return
        self.dma(q, tile[:, 0:n1], src[:, 0:n1], writes=[tile])
        nfree = 1
        for d_ in tile.t.shape[1:]:
            nfree *= d_
        cap = self.nc.dram_tensor("wc_%d" % len(self.wcache), [128, nfree], BF16).ap()
        cres = Res()
        self.wcache[key] = (cap, cres)
        self.dma("sp", cap, flat, reads=[tile], writes=[cres])

    def barrier(self):
        toks = [(E["sem"], E["cnt"]) for E in self.E.values() if E["cnt"] > 0]
        for Q in self.dq.values():
            toks += [(sl[0], sl[1]) for sl in Q["sl"] if sl[1] > 0]
        for e in self.E:
            self._wait(e, toks)


def build(T, C, NE, dbg=False, upto=99, modv_in=False):
    NG = NE // 4
    NTOK = T + C
    nc = bass.Bass("TRN2", target_bir_lowering=False)

    def din(name, shape):
        return nc.dram_tensor(name, list(shape), F32, kind="ExternalInput").ap()

    x_in = din("x", (T, D))
    c_in = din("c", (D,))
    ctx_in = din("ctx", (C, D))
    cctx_in = din("c_ctx", (D,))
    if modv_in:
        modv_src = din("modv_in", (2, 2, 6 * D))
    else:
        ada_w = din("ada_w", (2, D, 6 * D))
        ada_b = din("ada_b", (2, 6 * D))
    norm_g = din("norm_g", (2, 2, D))
    final_g = din("final_g", (D,))
    a_wqkv = din("a_wqkv", (D, 5120))
    a_wo = din("a_wo", (D, D))
    a_sink = din("a_sink", (64,))
    b_wdkv = din("b_wdkv", (D, 1600))
    b_gq = din("b_gq", (1024,))
    b_gkv = din("b_gkv", (512,))
    b_wuq = din("b_wuq", (1024, 6144))
    b_wukv = din("b_wukv", (512, 8192))
    b_wo = din("b_wo", (D, D))
    NR = NG + NE
    r_w = din("r_w", (2, D, NR))
    r_b = din("r_b", (2, NR))
    e_wgu = din("e_wgu", (2, NE, D, 1024))
    e_wdn = din("e_wdn", (2, NE, 512, D))
    k_ident = din("k_ident", (128, 128))
    k_rot = din("k_rot", (128, 128))
    k_cos = din("k_cos", (128, T))
    k_sin = din("k_sin", (128, T))
    k_mprev = din("k_mprev", (128, 512))
    k_mnext = din("k_mnext", (128, 512))
    out = nc.dram_tensor("out", [T, D], F32, kind="ExternalOutput").ap()

    def dscr(name, shape, dt):
        if dbg:
            return nc.dram_tensor(name, list(shape), dt, kind="ExternalOutput").ap()
        return nc.dram_tensor(name, list(shape), dt).ap()

    xres = dscr("xres", (NTOK, D), F32)
    modv = dscr("modv", (2, 2, 6 * D), F32)
    hT = dscr("hT", (D, NTOK), BF16)
    qT = dscr("qT", (D, NTOK), BF16)
    kT = dscr("kT", (512, NTOK), BF16)
    vv = dscr("vv", (NTOK, 512), BF16)
    oT = dscr("oT", (D, NTOK), BF16)
    zqT = dscr("zqT", (1024, T), BF16)
    ckvT = dscr("ckvT", (512, NTOK), BF16)
    krT = dscr("krT", (64, NTOK), BF16)
    qnT = dscr("qnT", (32 * 128, T), BF16)
    qrT = dscr("qrT", (32 * 64, T), BF16)

    st = ExitStack()
    with st:
        tr = Tr(nc, st)
        op, dma = tr.op, tr.dma

        uid = [0]

        def sb(ph, name, shape, dt):
            uid[0] += 1
            return Tile(ph.enter_context(nc.sbuf_tensor("%s_%d" % (name, uid[0]), list(shape), dt)))

        PS = [Tile(st.enter_context(nc.psum_tensor("ps%d" % i, [128, 512], F32))) for i in range(8)]
        for p_ in PS:
            p_.excl = True

        ident = sb(st, "ident", (128, 128), BF16)
        rot = sb(st, "rot", (128, 128), BF16)
        ones = sb(st, "ones", (128, 128), BF16)
        dma("pool", ident[:], k_ident, writes=[ident])
        dma("pool", rot[:], k_rot, writes=[rot])
        op("dve", lambda v: v.memset(ones[:], 1.0), writes=[ones])

        def stiles(with_ctx):
            L = [(t0, 512, False) for t0 in range(0, T, 512)]
            if with_ctx:
                L += [(T + c0, min(512, C - c0), True) for c0 in range(0, C, 512)]
            return L

        with ExitStack() as ph:
            dma("sp", xres[0:T, :], x_in)
            dma("sp", xres[T:NTOK, :], ctx_in)
            cs = sb(ph, "cs", (128, KC, 2), F32)
            csil = sb(ph, "csil", (128, KC, 2), F32)
            dma("sp", cs[:, :, 0], c_in.rearrange("(kc p) -> p kc", p=128), writes=[cs], allow_slow_non_contiguous=True)
            dma("sp", cs[:, :, 1], cctx_in.rearrange("(kc p) -> p kc", p=128), writes=[cs], allow_slow_non_contiguous=True)
            op("act", lambda a: a.activation(out=csil[:], in_=cs[:], func=AF.Silu), reads=[cs], writes=[csil])
            wts = [sb(ph, "adw%d" % i, (128, KC, 512), F32) for i in range(2)]
            bts = [sb(ph, "adb%d" % i, (2, 512), F32) for i in range(2)]
            mts = [sb(ph, "adm%d" % i, (2, 512), F32) for i in range(2)]
            n = 0
            if modv_in:
                dma("sp", modv, modv_src)
            for i in range(0 if modv_in else 2):
                for cb in range(48):
                    w = wts[n % 2]; bt = bts[n % 2]; mt = mts[n % 2]; ps = PS[n % 2]
                    n += 1
                    dma("sp", w[:], ada_w[i].rearrange("(kc p) n -> p kc n", p=128)[:, :, cb * 512:(cb + 1) * 512], writes=[w])
                    for r in range(2):
                        dma("pool", bt[r:r + 1, :], ada_b[i:i + 1, cb * 512:(cb + 1) * 512], writes=[bt])
                    for kc in range(KC):
                        op("pe", lambda t, kc=kc: t.matmul(ps[0:2, :], csil[:, kc, :], w[:, kc, :], start=(kc == 0), stop=(kc == KC - 1)),
                           reads=[csil, w], writes=[ps])
                    op("dve", lambda v: v.tensor_tensor(out=mt[:], in0=ps[0:2, :], in1=bt[:], op=ALU.add), reads=[ps, bt], writes=[mt])
                    dma("sp", modv[i, :, cb * 512:(cb + 1) * 512], mt[:], reads=[mt])
            tr.barrier()

        def load_rep(q, tile, src_row_ap):
            dma(q, tile[:], src_row_ap.partition_broadcast(128), writes=[tile])

        def phase_norm(layer, which, with_ctx):
            with ExitStack() as ph:
                A = [sb(ph, "nA%d" % r, (128, D), F32) for r in range(2)]
                B = [sb(ph, "nB%d" % r, (128, D), F32) for r in range(2)]
                gt = sb(ph, "ng", (128, D), F32)
                load_rep("sp", gt, norm_g[layer, which:which + 1, :])
                for r in range(2 if with_ctx else 1):
                    sh = (0 if which == 0 else 3) * D
                    load_rep("sp", B[r], modv[layer, r:r + 1, sh:sh + D])
                    load_rep("sp", A[r], modv[layer, r:r + 1, sh + D:sh + 2 * D])
                    op("dve", lambda v, r=r: v.scalar_tensor_tensor(out=A[r][:], in0=A[r][:], scalar=1.0, in1=gt[:], op0=ALU.add, op1=ALU.mult),
                       reads=[A[r], gt], writes=[A[r]])
                xts = [sb(ph, "nx%d" % i, (128, D), F32) for i in range(2)]
                sq = sb(ph, "nsq", (128, D), BF16)
                hb = [sb(ph, "nhb%d" % i, (128, D), BF16) for i in range(2)]
                hTt = [sb(ph, "nhT%d" % i, (128, KC, 128), BF16) for i in range(2)]
                ss = [sb(ph, "nss%d" % i, (128, 2), F32) for i in range(2)]
                ntile = (NTOK if with_ctx else T) // 128
                for t in range(ntile):
                    r = 1 if t * 128 >= T else 0
                    xt = xts[t % 2]; h = hb[t % 2]; hT_t = hTt[t % 2]; s_ = ss[t % 2]
                    dma("sp", xt[:], xres[t * 128:(t + 1) * 128, :], writes=[xt])
                    op("dve", lambda v: v.memset(s_[:], 0.0), writes=[s_])
                    op("act", lambda a: a.activation(out=sq[:], in_=xt[:], func=AF.Square, accum_out=s_[:, 0:1]), reads=[xt, s_], writes=[sq, s_])
                    op("dve", lambda v: v.tensor_scalar(out=s_[:, 1:2], in0=s_[:, 0:1], scalar1=1.0 / D, scalar2=EPS, op0=ALU.mult, op1=ALU.add), reads=[s_], writes=[s_])
                    op("act", lambda a: a.activation(out=s_[:, 1:2], in_=s_[:, 1:2], func=AF.Sqrt), reads=[s_], writes=[s_])
                    op("dve", lambda v: v.reciprocal(out=s_[:, 1:2], in_=s_[:, 1:2]), reads=[s_], writes=[s_])
                    op("dve", lambda v: v.scalar_tensor_tensor(out=xt[:], in0=xt[:], scalar=s_[:, 1:2], in1=A[r][:], op0=ALU.mult, op1=ALU.mult),
                       reads=[xt, s_, A[r]], writes=[xt])
                    op("pool", lambda g: g.tensor_tensor(out=h[:], in0=xt[:], in1=B[r][:], op=ALU.add), reads=[xt, B[r]], writes=[h])
                    for q4 in range(4):
                        ps = PS[(t * 4 + q4) % 4]
                        psb = ps[:].bitcast(BF16)
                        for j in range(8):
                            kc = q4 * 8 + j
                            op("pe", lambda te, kc=kc, j=j: te.transpose(psb[:, j * 128:(j + 1) * 128], h[:, kc * 128:(kc + 1) * 128], ident[:]),
                               reads=[h, ident], writes=[ps])
                        eng = "act" if q4 % 2 == 0 else "dve"
                        if eng == "act":
                            op("act", lambda a: a.copy(out=hT_t[:, q4 * 8:(q4 + 1) * 8, :], in_=psb.rearrange("p (j t) -> p j t", j=8)), reads=[ps], writes=[hT_t])
                        else:
                            op("dve", lambda v: v.tensor_copy(out=hT_t[:, q4 * 8:(q4 + 1) * 8, :], in_=psb.rearrange("p (j t) -> p j t", j=8)), reads=[ps], writes=[hT_t])
                    dma("sp", hT.rearrange("(kc p) t -> p kc t", p=128)[:, :, t * 128:(t + 1) * 128], hT_t[:], reads=[hT_t])
                tr.barrier()

        def rope_store(ph_tiles, ps, np_, W, t0, dst_ap):
            zb, t1, t2, ob, cs_t, sn_t, ps2 = ph_tiles
            op("act", lambda a: a.copy(out=zb[0:np_, 0:W], in_=ps[0:np_, 0:W]), reads=[ps], writes=[zb])
            op("pe", lambda te: te.matmul(ps2[0:np_, 0:W], rot[0:np_, 0:np_], zb[0:np_, 0:W], start=True, stop=True), reads=[rot, zb], writes=[ps2])
            op("dve", lambda v: v.tensor_tensor(out=t1[0:np_, 0:W], in0=ps[0:np_, 0:W], in1=cs_t[0:np_, 0:W], op=ALU.mult), reads=[ps, cs_t], writes=[t1])
            op("dve", lambda v: v.tensor_tensor(out=t2[0:np_, 0:W], in0=ps2[0:np_, 0:W], in1=sn_t[0:np_, 0:W], op=ALU.mult), reads=[ps2, sn_t], writes=[t2])
            op("pool", lambda g: g.tensor_tensor(out=ob[0:np_, 0:W], in0=t1[0:np_, 0:W], in1=t2[0:np_, 0:W], op=ALU.add), reads=[t1, t2], writes=[ob])
            dma("sp", dst_ap, ob[0:np_, 0:W], reads=[ob])

        def rope_tiles(ph):
            return [sb(ph, "rzb", (128, 512), BF16), sb(ph, "rt1", (128, 512), F32), sb(ph, "rt2", (128, 512), F32),
                    sb(ph, "rob", (128, 512), BF16), sb(ph, "rcs", (128, 512), F32), sb(ph, "rsn", (128, 512), F32), PS[7]]

        def phase_qkv():
            with ExitStack() as ph:
                hs = [sb(ph, "qh%d" % i, (128, KC, 512), BF16) for i in range(2)]
                ws = [sb(ph, "qw%d" % i, (128, KC, 512), BF16) for i in range(2)]
                rt = rope_tiles(ph)
                ob = sb(ph, "qob", (128, 512), BF16)
                nw = 0
                for si, (t0, W, isc) in enumerate(stiles(True)):
                    h = hs[si % 2]
                    dma("sp", h[:, :, 0:W], hT.rearrange("(kc p) t -> p kc t", p=128)[:, :, t0:t0 + W], writes=[h])
                    if not isc:
                        dma("sp", rt[4][:, 0:W], k_cos[:, t0:t0 + W], writes=[rt[4]])
                        dma("sp", rt[5][:, 0:W], k_sin[:, t0:t0 + W], writes=[rt[5]])
                    for cb in range(10):
                        w = ws[nw % 2]; nw += 1
                        tr.wload("pool", w, a_wqkv.rearrange("(kc p) n -> p kc n", p=128)[:, :, cb * 512:(cb + 1) * 512], KC, key=("qkv", cb))
                        if cb < 9:
                            for m in range(4):
                                ps = PS[m % 2]
                                for kc in range(KC):
                                    op("pe", lambda te, kc=kc: te.matmul(ps[:, 0:W], w[:, kc, m * 128:(m + 1) * 128], h[:, kc, 0:W], start=(kc == 0), stop=(kc == KC - 1)),
                                       reads=[w, h], writes=[ps])
                                row0 = cb * 512 + m * 128
                                dst = (qT[row0:row0 + 128, t0:t0 + W] if cb < 8 else kT[row0 - 4096:row0 - 4096 + 128, t0:t0 + W])
                                QM = int(os.environ.get("QKV_MODE", "9"))
                                if not isc and QM >= 2:
                                    rope_store(rt, ps, 128, W, t0, dst)
                                elif QM >= 1:
                                    op("act", lambda a: a.copy(out=ob[:, 0:W], in_=ps[:, 0:W]), reads=[ps], writes=[ob])
                                    dma("sp", dst, ob[:, 0:W], reads=[ob])
                        elif int(os.environ.get("QKV_MODE", "9")) >= 3:
                            for tt in range(W // 128):
                                ps = PS[2 + tt % 2]
                                for kc in range(KC):
                                    op("pe", lambda te, kc=kc: te.matmul(ps[:], h[:, kc, tt * 128:(tt + 1) * 128], w[:, kc, :], start=(kc == 0), stop=(kc == KC - 1)),
                                       reads=[w, h], writes=[ps])
                                op("act", lambda a: a.copy(out=ob[:], in_=ps[:]), reads=[ps], writes=[ob])
                                dma("sp", vv[t0 + tt * 128:t0 + (tt + 1) * 128, :], ob[:], reads=[ob])
                tr.barrier()

        def phase_att0():
            with ExitStack() as ph:
                mprev = sb(ph, "mprev", (128, 512), BF16)
                mnext = sb(ph, "mnext", (128, 512), BF16)
                dma("pool", mprev[:], k_mprev, writes=[mprev])
                dma("pool", mnext[:], k_mnext, writes=[mnext])
                sk = sb(ph, "sk", (1, 64), F32)
                es1 = sb(ph, "es1", (1, 64), F32)
                esrow = sb(ph, "esrow", (1, 64 * 128), BF16)
                dma("sp", sk[:], a_sink.rearrange("(o h) -> o h", o=1), writes=[sk])
                op("act", lambda a: a.activation(out=es1[:], in_=sk[:], func=AF.Exp), reads=[sk], writes=[es1])
                op("dve", lambda v: v.memset(esrow[:], 0.0), writes=[esrow])
                for hh in range(64):
                    op("dve", lambda v, hh=hh: v.tensor_scalar(out=esrow[:, hh * 128:(hh + 1) * 128], in0=esrow[:, hh * 128:(hh + 1) * 128], scalar1=es1[:, hh:hh + 1], scalar2=None, op0=ALU.add),
                       reads=[esrow, es1], writes=[esrow])
                kc_t = sb(ph, "kct", (64, C), BF16)
                vc_t = sb(ph, "vct", (128, C // 128, 64), BF16)
                qs = [sb(ph, "aq%d" % i, (64, 8, 512), BF16) for i in range(2)]
                ks = [sb(ph, "ak%d" % i, (64, 768), BF16) for i in range(2)]
                vs = [sb(ph, "av%d" % i, (128, 6, 64), BF16) for i in range(2)]
                pts = [sb(ph, "ap%d" % i, (128, 512), BF16) for i in range(3)]
                den = sb(ph, "aden", (64, 512), F32)
                obs = [sb(ph, "aob%d" % i, (64, 512), BF16) for i in range(2)]
                npt = 0
                nob = 0
                nJ = 0
                for g in range(8):
                    dma("sp", kc_t[:], kT[g * 64:(g + 1) * 64, T:T + C], writes=[kc_t])
                    dma("sp", vc_t[:], vv[T:T + C, g * 64:(g + 1) * 64].rearrange("(b p) d -> p b d", p=128), writes=[vc_t])
                    for (t0, W, isc) in stiles(True):
                        q = qs[nJ % 2]; k = ks[nJ % 2]; v = vs[nJ % 2]; nJ += 1
                        dma("sp", q[:, :, 0:W], qT[g * 512:(g + 1) * 512, t0:t0 + W].rearrange("(h d) t -> d h t", d=64), writes=[q])
                        if not isc:
                            k0 = max(t0 - 128, 0); k1 = min(t0 + W + 128, T)
                            koff = k0 - (t0 - 128)
                            dma("sp", k[:, koff:koff + (k1 - k0)], kT[g * 64:(g + 1) * 64, k0:k1], writes=[k])
                            dma("sp", v[:, koff // 128:koff // 128 + (k1 - k0) // 128, :],
                                vv[k0:k1, g * 64:(g + 1) * 64].rearrange("(b p) d -> p b d", p=128), writes=[v])
                        for jb in range(W // 128):
                            tq = t0 + jb * 128
                            kbl = []
                            if not isc:
                                if tq - 128 >= 0:
                                    kbl.append((k[:, jb * 128:(jb + 1) * 128], v[:, jb, :], mprev))
                                kbl.append((k[:, (jb + 1) * 128:(jb + 2) * 128], v[:, jb + 1, :], None))
                                if tq + 256 <= T:
                                    kbl.append((k[:, (jb + 2) * 128:(jb + 3) * 128], v[:, jb + 2, :], mnext))
                            for cbk in range(C // 128):
                                kbl.append((kc_t[:, cbk * 128:(cbk + 1) * 128], vc_t[:, cbk, :], None))
                            for a in range(2):
                                po = PS[4]; pd = PS[5]
                                rhs_q = q[:, a * 4:(a + 1) * 4, jb * 128:(jb + 1) * 128]
                                nkb_ = len(kbl)

                                def qk0(bi):
                                    kap, vap, msk = kbl[bi]
                                    ps = PS[bi % 3]
                                    op("pe", lambda te: te.matmul(ps[:].rearrange("p (h t) -> p h t", h=4), kap, rhs_q, start=True, stop=(msk is None)), reads=[k, kc_t, q], writes=[ps])
                                    if msk is not None:
                                        op("pe", lambda te: te.matmul(ps[:], ident[:], msk[:], start=False, stop=True), reads=[ident, msk], writes=[ps])

                                def rest0(bi, pt):
                                    kap, vap, msk = kbl[bi]
                                    ps = PS[bi % 3]
                                    op("act", lambda ac: ac.activation(out=pt[:], in_=ps[:], func=AF.Exp, scale=0.125), reads=[ps], writes=[pt])
                                    op("pe", lambda te: te.matmul(po[0:64, :], vap, pt[:], start=(bi == 0), stop=(bi == nkb_ - 1)), reads=[v, vc_t, pt], writes=[po])
                                    op("pe", lambda te: te.matmul(pd[0:64, :], ones[:, 0:64], pt[:], start=(bi == 0), stop=False), reads=[ones, pt], writes=[pd])

                                for bi in range(min(2, nkb_)):
                                    qk0(bi)
                                for bi in range(nkb_):
                                    if bi + 2 < nkb_:
                                        qk0(bi + 2)
                                    rest0(bi, pts[npt % 3])
                                    npt += 1
                                op("pe", lambda te: te.matmul(pd[0:64, :], ones[0:1, 0:64], esrow[0:1, (g * 8 + a * 4) * 128:(g * 8 + a * 4 + 4) * 128], start=False, stop=True),
                                   reads=[ones, esrow], writes=[pd])
                                ob = obs[nob % 2]; nob += 1
                                op("dve", lambda vE: vE.reciprocal(out=den[:], in_=pd[0:64, :]), reads=[pd], writes=[den])
                                op("dve", lambda vE: vE.tensor_tensor(out=ob[:], in0=po[0:64, :], in1=den[:], op=ALU.mult), reads=[po, den], writes=[ob])
                                h0 = g * 8 + a * 4
                                dma("sp", oT[h0 * 64:(h0 + 4) * 64, tq:tq + 128].rearrange("(h d) t -> d h t", d=64),
                                    ob[:].rearrange("d (h t) -> d h t", h=4), reads=[ob])
                tr.barrier()

        def phase_wo(layer, wo, with_ctx):
            with ExitStack() as ph:
                G = [sb(ph, "wG%d" % r, (128, D), F32) for r in range(2)]
                for r in range(2 if with_ctx else 1):
                    load_rep("sp", G[r], modv[layer, r:r + 1, 2 * D:3 * D])
                os_ = [sb(ph, "wo_o%d" % i, (128, KC, 512), BF16) for i in range(2)]
                ws = [sb(ph, "wo_w%d" % i, (128, KC, 512), BF16) for i in range(2)]
                xb = [sb(ph, "wo_x%d" % i, (128, 512), F32) for i in range(3)]
                tb = [sb(ph, "wo_t%d" % i, (128, 512), F32) for i in range(3)]
                nw = 0; nx = 0
                for si, (t0, W, isc) in enumerate(stiles(with_ctx)):
                    o = os_[si % 2]
                    r = 1 if isc else 0
                    dma("sp", o[:, :, 0:W], oT.rearrange("(kc p) t -> p kc t", p=128)[:, :, t0:t0 + W], writes=[o])
                    for cb in range(8):
                        w = ws[nw % 2]; nw += 1
                        tr.wload("pool", w, wo.rearrange("(kc p) n -> p kc n", p=128)[:, :, cb * 512:(cb + 1) * 512], KC, key=("wo", layer, cb))
                        for tt in range(W // 128):
                            ps = PS[tt % 4]
                            x_ = xb[nx % 3]; t_ = tb[nx % 3]; nx += 1
                            rows = slice(t0 + tt * 128, t0 + (tt + 1) * 128)
                            dma("sp", x_[:], xres[rows, cb * 512:(cb + 1) * 512], writes=[x_])
                            for kc in range(KC):
                                op("pe", lambda te, kc=kc: te.matmul(ps[:], o[:, kc, tt * 128:(tt + 1) * 128], w[:, kc, :], start=(kc == 0), stop=(kc == KC - 1)),
                                   reads=[o, w], writes=[ps])
                            op("dve", lambda v: v.tensor_tensor(out=t_[:], in0=ps[:], in1=G[r][:, cb * 512:(cb + 1) * 512], op=ALU.mult), reads=[ps, G[r]], writes=[t_])
                            op("pool", lambda g_: g_.tensor_tensor(out=t_[:], in0=t_[:], in1=x_[:], op=ALU.add), reads=[t_, x_], writes=[t_])
                            dma("sp", xres[rows, cb * 512:(cb + 1) * 512], t_[:], reads=[t_])
                tr.barrier()

        def phase_moe(layer, with_ctx):
            with ExitStack() as ph:
                G = [sb(ph, "mG%d" % r, (128, D), F32) for r in range(2)]
                for r in range(2 if with_ctx else 1):
                    load_rep("sp", G[r], modv[layer, r:r + 1, 5 * D:6 * D])
                wr = sb(ph, "m_wr", (128, KC, NR), BF16)
                rb = sb(ph, "m_rb", (1, NR), BF16)
                tr.wload("pool", wr, r_w[layer].rearrange("(kc p) n -> p kc n", p=128), KC)
                dma("pool", rb[:], r_b[layer:layer + 1, :], writes=[rb])
                hs = sb(ph, "m_h", (128, KC, 512), BF16)
                wg = [sb(ph, "m_wg%d" % i, (128, KC, 256), BF16) for i in range(2)]
                wd = [sb(ph, "m_wd%d" % i, (128, 4, 1024), BF16) for i in range(2)]
                yacc = sb(ph, "m_y", (128, 4, D), F32)
                yres = [[Res() for _ in range(8)] for _ in range(4)]
                dn_ps = [PS[4], PS[5], PS[7]]
                ndn = 0
                sg = sb(ph, "m_sg", (128, 2, 512), F32)
                actT = sb(ph, "m_act", (128, 4, 512), BF16)
                gate = sb(ph, "m_gate", (128, 4, NE), F32)
                lg = sb(ph, "m_lg", (128, NR), F32)
                sm = sb(ph, "m_sm", (128, 8 + 4 * NG), F32)
                o1, o2, o3, o4 = 8, 8 + NG, 8 + 2 * NG, 8 + 3 * NG
                t8 = sb(ph, "m_t8", (128, NG), F32)
                m8 = sb(ph, "m_m8", (128, NG), F32)
                e32 = sb(ph, "m_e32", (128, NE), F32)
                k32 = sb(ph, "m_k32", (128, NE), F32)
                xb = [sb(ph, "m_x%d" % i, (128, 512), F32) for i in range(2)]
                tb = [sb(ph, "m_t%d" % i, (128, 512), F32) for i in range(2)]
                nwg = 0; nwd = 0; nx = 0
                for si, (t0, W, isc) in enumerate(stiles(with_ctx)):
                    r = 1 if isc else 0
                    ntt = W // 128
                    dma("sp", hs[:, :, 0:W], hT.rearrange("(kc p) t -> p kc t", p=128)[:, :, t0:t0 + W], writes=[hs])
                    for tt in range(ntt):
                        ps = PS[6]
                        for kc in range(KC):
                            op("pe", lambda te, kc=kc: te.matmul(ps[:, 0:NR], hs[:, kc, tt * 128:(tt + 1) * 128], wr[:, kc, :], start=(kc == 0), stop=False), reads=[hs, wr], writes=[ps])
                        op("pe", lambda te: te.matmul(ps[:, 0:NR], ones[0:1, :], rb[:], start=False, stop=True), reads=[ones, rb], writes=[ps])
                        op("dve", lambda v: v.tensor_copy(out=lg[:], in_=ps[:, 0:NR]), reads=[ps], writes=[lg])
                        op("dve", lambda v: v.tensor_reduce(out=sm[:, 0:1], in_=lg[:, 0:NG], axis=AX.X, op=ALU.max), reads=[lg], writes=[sm])
                        op("dve", lambda v: v.tensor_scalar(out=m8[:], in0=lg[:, 0:NG], scalar1=sm[:, 0:1], scalar2=None, op0=ALU.is_ge), reads=[lg, sm], writes=[m8])
                        op("dve", lambda v: v.tensor_scalar(out=t8[:], in0=lg[:, 0:NG], scalar1=sm[:, 0:1], scalar2=None, op0=ALU.subtract), reads=[lg, sm], writes=[t8])
                        op("dve", lambda v: v.memset(sm[:, 1:2], 0.0), writes=[sm])
                        op("act", lambda a: a.activation(out=t8[:], in_=t8[:], func=AF.Exp, accum_out=sm[:, 1:2]), reads=[t8, sm], writes=[t8, sm])
                        op("dve", lambda v: v.reciprocal(out=sm[:, 2:3], in_=sm[:, 1:2]), reads=[sm], writes=[sm])
                        le = lg[:, NG:NR].rearrange("p (g e) -> p g e", e=4)
                        op("dve", lambda v: v.tensor_reduce(out=sm[:, o1:o2], in_=le, axis=AX.X, op=ALU.max), reads=[lg], writes=[sm])
                        mx1 = sm[:, o1:o2].unsqueeze(2).to_broadcast([128, NG, 4])
                        k3 = k32[:].rearrange("p (g e) -> p g e", e=4)
                        e3 = e32[:].rearrange("p (g e) -> p g e", e=4)
                        op("dve", lambda v: v.tensor_tensor(out=k3, in0=le, in1=mx1, op=ALU.is_ge), reads=[lg, sm], writes=[k32])
                        op("dve", lambda v: v.tensor_tensor(out=e3, in0=le, in1=mx1, op=ALU.subtract), reads=[lg, sm], writes=[e32])
                        op("dve", lambda v: v.scalar_tensor_tensor(out=k32[:], in0=k32[:], scalar=-1e9, in1=e32[:], op0=ALU.mult, op1=ALU.add), reads=[k32, e32], writes=[k32])
                        op("dve", lambda v: v.tensor_reduce(out=sm[:, o2:o3], in_=k3, axis=AX.X, op=ALU.max), reads=[k32], writes=[sm])
                        mx2 = sm[:, o2:o3].unsqueeze(2).to_broadcast([128, NG, 4])
                        op("dve", lambda v: v.tensor_tensor(out=k3, in0=e3, in1=mx2, op=ALU.is_ge), reads=[e32, sm], writes=[k32])
                        op("act", lambda a: a.activation(out=e32[:], in_=e32[:], func=AF.Exp), reads=[e32], writes=[e32])
                        op("dve", lambda v: v.tensor_tensor(out=e32[:], in0=e32[:], in1=k32[:], op=ALU.mult), reads=[e32, k32], writes=[e32])
                        op("dve", lambda v: v.tensor_reduce(out=sm[:, o3:o4], in_=e3, axis=AX.X, op=ALU.add), reads=[e32], writes=[sm])
                        op("dve", lambda v: v.reciprocal(out=sm[:, o4:o4 + NG], in_=sm[:, o3:o4]), reads=[sm], writes=[sm])
                        op("dve", lambda v: v.tensor_tensor(out=m8[:], in0=m8[:], in1=sm[:, o4:o4 + NG], op=ALU.mult), reads=[m8, sm], writes=[m8])
                        op("dve", lambda v: v.tensor_scalar(out=m8[:], in0=m8[:], scalar1=sm[:, 2:3], scalar2=None, op0=ALU.mult), reads=[m8, sm], writes=[m8])
                        op("dve", lambda v: v.tensor_tensor(out=gate[:, tt, :].rearrange("p (g e) -> p g e", e=4), in0=e3, in1=m8[:].unsqueeze(2).to_broadcast([128, NG, 4]), op=ALU.mult),
                           reads=[e32, m8], writes=[gate])
                    for e in range(NE):
                        for half in range(2):
                            for gu in range(2):
                                w = wg[nwg % 2]; nwg += 1
                                c0 = gu * 512 + half * 256
                                tr.wload("pool", w, e_wgu[layer, e].rearrange("(kc p) n -> p kc n", p=128)[:, :, c0:c0 + 256], KC, key=("gu", layer, e, c0))
                                for m in range(2):
                                    ps = PS[(gu * 2 + m) % 4]
                                    for kc in range(KC):
                                        op("pe", lambda te, kc=kc: te.matmul(ps[:, 0:W], w[:, kc, m * 128:(m + 1) * 128], hs[:, kc, 0:W], start=(kc == 0), stop=(kc == KC - 1)),
                                           reads=[w, hs], writes=[ps])
                                    if gu == 0:
                                        op("act", lambda a: a.activation(out=sg[:, m, 0:W], in_=ps[:, 0:W], func=AF.Silu), reads=[ps], writes=[sg])
                                    else:
                                        op("dve", lambda v: v.tensor_tensor(out=actT[:, half * 2 + m, 0:W], in0=ps[:, 0:W], in1=sg[:, m, 0:W], op=ALU.mult), reads=[ps, sg], writes=[actT])
                        for dh in range(4):
                            w = wd[nwd % 2]; nwd += 1
                            tr.wload("pool", w, e_wdn[layer, e].rearrange("(kc p) n -> p kc n", p=128)[:, :, dh * 1024:(dh + 1) * 1024], 4, key=("dn", layer, e, dh))
                            for tt in range(ntt):
                                for cbl in range(2):
                                    cb = dh * 2 + cbl
                                    ps = dn_ps[ndn % 3]; ndn += 1
                                    yr = yres[tt][cb]
                                    for kc in range(4):
                                        op("pe", lambda te, kc=kc: te.matmul(ps[:], actT[:, kc, tt * 128:(tt + 1) * 128], w[:, kc, cbl * 512:(cbl + 1) * 512], start=(kc == 0), stop=(kc == 3)),
                                           reads=[actT, w], writes=[ps])
                                    ysl = yacc[:, tt, cb * 512:(cb + 1) * 512]
                                    if e == 0:
                                        op("dve", lambda v: v.tensor_scalar(out=ysl, in0=ps[:], scalar1=gate[:, tt, e:e + 1], scalar2=None, op0=ALU.mult), reads=[ps, gate], writes=[yr])
                                    else:
                                        op("dve", lambda v: v.scalar_tensor_tensor(out=ysl, in0=ps[:], scalar=gate[:, tt, e:e + 1], in1=ysl, op0=ALU.mult, op1=ALU.add), reads=[ps, gate, yr], writes=[yr])
                    for tt in range(ntt):
                        rows = slice(t0 + tt * 128, t0 + (tt + 1) * 128)
                        for cb in range(8):
                            x_ = xb[nx % 2]; t_ = tb[nx % 2]; nx += 1
                            dma("sp", x_[:], xres[rows, cb * 512:(cb + 1) * 512], writes=[x_])
                            op("pool", lambda g_: g_.tensor_tensor(out=t_[:], in0=yacc[:, tt, cb * 512:(cb + 1) * 512], in1=G[r][:, cb * 512:(cb + 1) * 512], op=ALU.mult), reads=[yres[tt][cb], G[r]], writes=[t_])
                            op("pool", lambda g_: g_.tensor_tensor(out=t_[:], in0=t_[:], in1=x_[:], op=ALU.add), reads=[t_, x_], writes=[t_])
                            dma("sp", xres[rows, cb * 512:(cb + 1) * 512], t_[:], reads=[t_])
                tr.barrier()

        def phase_dkv():
            with ExitStack() as ph:
                hs = [sb(ph, "dh%d" % i, (128, KC, 512), BF16) for i in range(2)]
                ws = [sb(ph, "dw%d" % i, (128, KC, 512), BF16) for i in range(2)]
                rt = rope_tiles(ph)
                zf = sb(ph, "dzf", (128, 12, 512), F32)
                sqb = [sb(ph, "dsq%d" % i, (128, 512), BF16) for i in range(2)]
                rstd = sb(ph, "drs", (128, 512), F32)
                gq = sb(ph, "dgq", (128, 8), F32)
                gkv = sb(ph, "dgkv", (128, 4), F32)
                ob = [sb(ph, "dob%d" % i, (128, 512), BF16) for i in range(2)]
                dma("sp", gq[:], b_gq.rearrange("(c p) -> p c", p=128), writes=[gq], allow_slow_non_contiguous=True)
                dma("sp", gkv[:], b_gkv.rearrange("(c p) -> p c", p=128), writes=[gkv], allow_slow_non_contiguous=True)
                nw = 0; nsq = 0; nob = 0
                for si, (t0, W, isc) in enumerate(stiles(True)):
                    h = hs[si % 2]
                    dma("sp", h[:, :, 0:W], hT.rearrange("(kc p) t -> p kc t", p=128)[:, :, t0:t0 + W], writes=[h])
                    if not isc:
                        dma("sp", rt[4][:, 0:W], k_cos[:, t0:t0 + W], writes=[rt[4]])
                        dma("sp", rt[5][:, 0:W], k_sin[:, t0:t0 + W], writes=[rt[5]])
                    for cb in range(4):
                        if cb < 2 and isc:
                            continue
                        w = ws[nw % 2]; nw += 1
                        ncol = 512 if cb < 3 else 64
                        tr.wload("pool", (w if ncol == 512 else Tview(w, ncol)), b_wdkv.rearrange("(kc p) n -> p kc n", p=128)[:, :, cb * 512:cb * 512 + ncol], KC, key=(("dkv", cb) if ncol == 512 else None))
                        for m in range(4 if cb < 3 else 1):
                            mw = 128 if cb < 3 else 64
                            ps = PS[m % 2]
                            for kc in range(KC):
                                op("pe", lambda te, kc=kc: te.matmul(ps[0:mw, 0:W], w[:, kc, m * 128:m * 128 + mw], h[:, kc, 0:W], start=(kc == 0), stop=(kc == KC - 1)),
                                   reads=[w, h], writes=[ps])
                            if cb < 3:
                                ci = cb * 4 + m
                                sq_ = sqb[nsq % 2]; nsq += 1
                                first = (ci in (0, 8)); last = (ci in (7, 11))
                                pss = PS[2] if ci < 8 else PS[3]
                                op("act", lambda a: a.activation(out=sq_[:, 0:W], in_=ps[:, 0:W], func=AF.Square), reads=[ps], writes=[sq_])
                                op("dve", lambda v: v.tensor_copy(out=zf[:, ci, 0:W], in_=ps[:, 0:W]), reads=[ps], writes=[zf])
                                op("pe", lambda te: te.matmul(pss[:, 0:W], ones[:], sq_[:, 0:W], start=first, stop=last), reads=[ones, sq_], writes=[pss])
                                if last:
                                    n_ = 1024.0 if ci == 7 else 512.0
                                    op("dve", lambda v: v.tensor_scalar(out=rstd[:, 0:W], in0=pss[:, 0:W], scalar1=1.0 / n_, scalar2=EPS, op0=ALU.mult, op1=ALU.add), reads=[pss], writes=[rstd])
                                    op("act", lambda a: a.activation(out=rstd[:, 0:W], in_=rstd[:, 0:W], func=AF.Sqrt), reads=[rstd], writes=[rstd])
                                    op("dve", lambda v: v.reciprocal(out=rstd[:, 0:W], in_=rstd[:, 0:W]), reads=[rstd], writes=[rstd])
                                    for c2 in (range(8) if ci == 7 else range(8, 12)):
                                        o_ = ob[nob % 2]; nob += 1
                                        gcol = gq[:, c2:c2 + 1] if c2 < 8 else gkv[:, c2 - 8:c2 - 7]
                                        op("dve", lambda v: v.scalar_tensor_tensor(out=o_[:, 0:W], in0=zf[:, c2, 0:W], scalar=gcol, in1=rstd[:, 0:W], op0=ALU.mult, op1=ALU.mult),
                                           reads=[zf, gq, gkv, rstd], writes=[o_])
                                        dst = zqT[c2 * 128:(c2 + 1) * 128, t0:t0 + W] if c2 < 8 else ckvT[(c2 - 8) * 128:(c2 - 7) * 128, t0:t0 + W]
                                        dma("sp", dst, o_[:, 0:W], reads=[o_])
                            else:
                                if not isc:
                                    rope_store(rt, ps, 64, W, t0, krT[:, t0:t0 + W])
                                else:
                                    o_ = ob[nob % 2]; nob += 1
                                    op("act", lambda a: a.copy(out=o_[0:64, 0:W], in_=ps[0:64, 0:W]), reads=[ps], writes=[o_])
                                    dma("sp", krT[:, t0:t0 + W], o_[0:64, 0:W], reads=[o_])
                tr.barrier()

        def phase_uq():
            with ExitStack() as ph:
                zs = [sb(ph, "uz%d" % i, (128, 8, 512), BF16) for i in range(2)]
                ws = [sb(ph, "uw%d" % i, (128, 8, 768), BF16) for i in range(2)]
                rt = rope_tiles(ph)
                ob = [sb(ph, "uob%d" % i, (128, 512), BF16) for i in range(2)]
                nw = 0; nob = 0
                for si, (t0, W, isc) in enumerate(stiles(False)):
                    z = zs[si % 2]
                    dma("sp", z[:, :, 0:W], zqT.rearrange("(kc p) t -> p kc t", p=128)[:, :, t0:t0 + W], writes=[z])
                    dma("sp", rt[4][:, 0:W], k_cos[:, t0:t0 + W], writes=[rt[4]])
                    dma("sp", rt[5][:, 0:W], k_sin[:, t0:t0 + W], writes=[rt[5]])
                    for hg in range(8):
                        w = ws[nw % 2]; nw += 1
                        tr.wload("pool", w, b_wuq.rearrange("(kc p) n -> p kc n", p=128)[:, :, hg * 768:(hg + 1) * 768], 8, key=("uq", hg))
                        for hl in range(4):
                            hd = hg * 4 + hl
                            ps = PS[hl % 2]
                            for kc in range(8):
                                op("pe", lambda te, kc=kc: te.matmul(ps[:, 0:W], w[:, kc, hl * 192:hl * 192 + 128], z[:, kc, 0:W], start=(kc == 0), stop=(kc == 7)), reads=[w, z], writes=[ps])
                            o_ = ob[nob % 2]; nob += 1
                            op("act", lambda a: a.copy(out=o_[:, 0:W], in_=ps[:, 0:W]), reads=[ps], writes=[o_])
                            dma("sp", qnT[hd * 128:(hd + 1) * 128, t0:t0 + W], o_[:, 0:W], reads=[o_])
                            ps = PS[2 + hl % 2]
                            for kc in range(8):
                                op("pe", lambda te, kc=kc: te.matmul(ps[0:64, 0:W], w[:, kc, hl * 192 + 128:hl * 192 + 192], z[:, kc, 0:W], start=(kc == 0), stop=(kc == 7)), reads=[w, z], writes=[ps])
                            rope_store(rt, ps, 64, W, t0, qrT[hd * 64:(hd + 1) * 64, t0:t0 + W])
                tr.barrier()

        def phase_mla():
            NKB = NTOK // 128
            sc = (128 + 64) ** -0.5
            with ExitStack() as ph:
                ckv = sb(ph, "l_ckv", (128, 4, NTOK), BF16)
                kr = sb(ph, "l_kr", (64, NTOK), BF16)
                dma("sp", ckv[:], ckvT.rearrange("(kc p) t -> p kc t", p=128), writes=[ckv])
                dma("sp", kr[:], krT, writes=[kr])
                wk = [sb(ph, "l_wk%d" % i, (128, 4, 256), BF16) for i in range(2)]
                kn = sb(ph, "l_kn", (128, NTOK), BF16)
                vh = sb(ph, "l_vh", (128, NKB, 128), BF16)
                qn = [sb(ph, "l_qn%d" % i, (128, 512), BF16) for i in range(2)]
                qr = [sb(ph, "l_qr%d" % i, (64, 512), BF16) for i in range(2)]
                pts = [sb(ph, "l_p%d" % i, (128, 512), BF16) for i in range(3)]
                den = sb(ph, "l_den", (128, 512), F32)
                obs = [sb(ph, "l_ob%d" % i, (128, 512), BF16) for i in range(2)]
                nq = 0; npt = 0; nob = 0; nps = 0
                for hd in range(32):
                    w = wk[hd % 2]
                    tr.wload("pool", w, b_wukv.rearrange("(kc p) n -> p kc n", p=128)[:, :, hd * 256:(hd + 1) * 256], 4)
                    for k0 in range(0, NTOK, 512):
                        kw = min(512, NTOK - k0)
                        ps = PS[nps % 2]; nps += 1
                        for kc in range(4):
                            op("pe", lambda te, kc=kc: te.matmul(ps[:, 0:kw], w[:, kc, 0:128], ckv[:, kc, k0:k0 + kw], start=(kc == 0), stop=(kc == 3)), reads=[w, ckv], writes=[ps])
                        op("dve", lambda v: v.tensor_copy(out=kn[:, k0:k0 + kw], in_=ps[:, 0:kw]), reads=[ps], writes=[kn])
                    for kb0 in range(0, NKB, 4):
                        nb = min(4, NKB - kb0)
                        ps = PS[nps % 2]; nps += 1
                        for bi in range(nb):
                            kb = kb0 + bi
                            for kc in range(4):
                                op("pe", lambda te, kc=kc: te.matmul(ps[:, bi * 128:(bi + 1) * 128], ckv[:, kc, kb * 128:(kb + 1) * 128], w[:, kc, 128:256], start=(kc == 0), stop=(kc == 3)), reads=[w, ckv], writes=[ps])
                        op("act", lambda a: a.copy(out=vh[:, kb0:kb0 + nb, :], in_=ps[:, 0:nb * 128].rearrange("p (b d) -> p b d", d=128)), reads=[ps], writes=[vh])
                    for (t0, W, isc) in stiles(False):
                        qn_ = qn[nq % 2]; qr_ = qr[nq % 2]; nq += 1
                        dma("pool", qn_[:, 0:W], qnT[hd * 128:(hd + 1) * 128, t0:t0 + W], writes=[qn_])
                        dma("pool", qr_[:, 0:W], qrT[hd * 64:(hd + 1) * 64, t0:t0 + W], writes=[qr_])
                        po = PS[5]; pd = PS[6]
                        def qk(kb):
                            ps = PS[2 + kb % 3]
                            op("pe", lambda te: te.matmul(ps[:, 0:W], kn[:, kb * 128:(kb + 1) * 128], qn_[:, 0:W], start=True, stop=False), reads=[kn, qn_], writes=[ps])
                            op("pe", lambda te: te.matmul(ps[:, 0:W], kr[:, kb * 128:(kb + 1) * 128], qr_[:, 0:W], start=False, stop=True), reads=[kr, qr_], writes=[ps])

                        def rest(kb, pt):
                            ps = PS[2 + kb % 3]
                            op("act", lambda a: a.activation(out=pt[:, 0:W], in_=ps[:, 0:W], func=AF.Exp, scale=sc), reads=[ps], writes=[pt])
                            op("pe", lambda te: te.matmul(po[:, 0:W], vh[:, kb, :], pt[:, 0:W], start=(kb == 0), stop=(kb == NKB - 1)), reads=[vh, pt], writes=[po])
                            op("pe", lambda te: te.matmul(pd[:, 0:W], ones[:], pt[:, 0:W], start=(kb == 0), stop=(kb == NKB - 1)), reads=[ones, pt], writes=[pd])

                        for kb in range(min(2, NKB)):
                            qk(kb)
                        for kb in range(NKB):
                            if kb + 2 < NKB:
                                qk(kb + 2)
                            rest(kb, pts[npt % 3])
                            npt += 1
                        ob = obs[nob % 2]; nob += 1
                        op("dve", lambda v: v.reciprocal(out=den[:, 0:W], in_=pd[:, 0:W]), reads=[pd], writes=[den])
                        op("dve", lambda v: v.tensor_tensor(out=ob[:, 0:W], in0=po[:, 0:W], in1=den[:, 0:W], op=ALU.mult), reads=[po, den], writes=[ob])
                        dma("sp", oT[hd * 128:(hd + 1) * 128, t0:t0 + W], ob[:, 0:W], reads=[ob])
                tr.barrier()

        def phase_final():
            with ExitStack() as ph:
                gt = sb(ph, "fg", (128, D), F32)
                load_rep("sp", gt, final_g.rearrange("(o d) -> o d", o=1))
                xts = [sb(ph, "fx%d" % i, (128, D), F32) for i in range(2)]
                sq = sb(ph, "fsq", (128, D), BF16)
                ss = [sb(ph, "fss%d" % i, (128, 2), F32) for i in range(2)]
                for t in range(T // 128):
                    xt = xts[t % 2]; s_ = ss[t % 2]
                    dma("sp", xt[:], xres[t * 128:(t + 1) * 128, :], writes=[xt])
                    op("dve", lambda v: v.memset(s_[:], 0.0), writes=[s_])
                    op("act", lambda a: a.activation(out=sq[:], in_=xt[:], func=AF.Square, accum_out=s_[:, 0:1]), reads=[xt, s_], writes=[sq, s_])
                    op("dve", lambda v: v.tensor_scalar(out=s_[:, 1:2], in0=s_[:, 0:1], scalar1=1.0 / D, scalar2=EPS, op0=ALU.mult, op1=ALU.add), reads=[s_], writes=[s_])
                    op("act", lambda a: a.activation(out=s_[:, 1:2], in_=s_[:, 1:2], func=AF.Sqrt), reads=[s_], writes=[s_])
                    op("dve", lambda v: v.reciprocal(out=s_[:, 1:2], in_=s_[:, 1:2]), reads=[s_], writes=[s_])
                    op("dve", lambda v: v.scalar_tensor_tensor(out=xt[:], in0=xt[:], scalar=s_[:, 1:2], in1=gt[:], op0=ALU.mult, op1=ALU.mult), reads=[xt, s_, gt], writes=[xt])
                    dma("sp", out[t * 128:(t + 1) * 128, :], xt[:], reads=[xt])
                tr.barrier()

        prog = [lambda: phase_norm(0, 0, True), phase_qkv, phase_att0, lambda: phase_wo(0, a_wo, True),
                lambda: phase_norm(0, 1, True), lambda: phase_moe(0, True), lambda: phase_norm(1, 0, True),
                phase_dkv, phase_uq, phase_mla, lambda: phase_wo(1, b_wo, False), lambda: phase_norm(1, 1, False),
                lambda: phase_moe(1, False), phase_final]
        for f in prog[:upto]:
            f()
    return nc


def host_consts(T, GRID_W=64):
    rows = T // GRID_W
    row = np.broadcast_to(np.arange(rows, dtype=np.float32)[:, None], (rows, GRID_W)).reshape(-1)
    col = np.broadcast_to(np.arange(GRID_W, dtype=np.float32)[None, :], (rows, GRID_W)).reshape(-1)
    n_freq = 16
    inv = (np.float32(10000.0) ** (-np.arange(n_freq, dtype=np.float32) / np.float32(n_freq))).astype(np.float32)
    ang = np.concatenate([row[:, None] * inv, col[:, None] * inv], axis=-1).astype(np.float32)
    cos = np.cos(ang).astype(np.float32).T
    sin = np.sin(ang).astype(np.float32).T
    k_cos = np.ascontiguousarray(np.tile(cos, (4, 1)))
    k_sin = np.ascontiguousarray(np.tile(sin, (4, 1)))
    rotT = np.zeros((128, 128), np.float32)
    for blk in (0, 64):
        for m in range(32):
            rotT[blk + m + 32, blk + m] = -1.0
            rotT[blk + m, blk + m + 32] = 1.0
    p = np.arange(128)[:, None]
    i = np.arange(128)[None, :]
    mprev = np.where(p >= i, 0.0, NEGM).astype(np.float32)
    mnext = np.where(p <= i, 0.0, NEGM).astype(np.float32)
    return dict(k_ident=np.eye(128, dtype=np.float32), k_rot=rotT, k_cos=k_cos, k_sin=k_sin,
                k_mprev=np.ascontiguousarray(np.tile(mprev, (1, 4))), k_mnext=np.ascontiguousarray(np.tile(mnext, (1, 4))))


def make_in_maps(inp, T, C):
    B = inp["x"].shape[0]
    hc = host_consts(T)
    r_w = np.ascontiguousarray(np.concatenate([inp["r_wg"], inp["r_we"]], axis=-1))
    r_b = np.ascontiguousarray(np.concatenate([inp["r_bg"], inp["r_be"]], axis=-1))
    maps = []
    for b in range(B):
        m = dict(x=inp["x"][b], c=inp["c"][b], ctx=inp["ctx"][b], c_ctx=inp["c_ctx"], ada_w=inp["ada_w"], ada_b=inp["ada_b"],
                 norm_g=inp["norm_g"], final_g=inp["final_g"], a_wqkv=inp["a_wqkv"][0], a_wo=inp["a_wo"][0], a_sink=inp["a_sink"][0],
                 b_wdkv=inp["b_wdkv"][0], b_gq=inp["b_gq"][0], b_gkv=inp["b_gkv"][0], b_wuq=inp["b_wuq"][0], b_wukv=inp["b_wukv"][0],
                 b_wo=inp["b_wo"][0], r_w=r_w, r_b=r_b, e_wgu=inp["e_wgu"], e_wdn=inp["e_wdn"])
        m.update(hc)
        maps.append({k: np.ascontiguousarray(np.asarray(v, dtype=np.float32)) for k, v in m.items()})
    return maps


def kernel(**inputs):
    inp = {k: np.asarray(v) for k, v in inputs.items()}
    B, T, _ = inp["x"].shape
    C = inp["ctx"].shape[1]
    NE = inp["e_wgu"].shape[1]
    nc = build(T, C, NE)
    maps = make_in_maps(inp, T, C)
    res = run_bass_kernel_spmd(nc, maps, core_ids=list(range(B)))
    return np.stack([res.results[b]["out"] for b in range(B)], axis=0).astype(np.float32)
```
